# Optimizing a Trainium2 kernel written in Bass

```python
import jax, jax.numpy as jnp
from jax import lax
import numpy as np

D_MODEL = 1024
BATCH = 16
SEQ = 2048
DEPTH = 4

PLE_DIM = 256
N_MIXERS = 2
EPS = 1e-6

GLA_HEADS = 4
GLA_KEY_DIM = D_MODEL // 2
GLA_VAL_DIM = D_MODEL
GLA_DK = GLA_KEY_DIM // GLA_HEADS
GLA_DV = GLA_VAL_DIM // GLA_HEADS
GLA_GATE_RANK = 16
GLA_GATE_NORMALIZER = 16.0
GLA_CHUNK = 64
GLA_IN = 2 * GLA_KEY_DIM + 2 * GLA_VAL_DIM + GLA_GATE_RANK

FOX_HEADS = 16
FOX_WIDTH = D_MODEL
FOX_HD = FOX_WIDTH // FOX_HEADS
FOX_BLOCK = 128
FOX_IN = 4 * FOX_WIDTH + FOX_HEADS

N_GLA = (DEPTH + 1) // 2
N_FOX = DEPTH // 2

kernel_name = "gla_fox_interleaved_hybrid"


def rms_norm(x, g):
    xf = x.astype(jnp.float32)
    y = xf * lax.rsqrt(jnp.mean(xf * xf, axis=-1, keepdims=True) + EPS)
    return (y * g.astype(jnp.float32)).astype(x.dtype)


def to_heads(t, n_heads):
    b, s, w = t.shape
    return t.reshape(b, s, n_heads, w // n_heads).transpose(0, 2, 1, 3)


def gla_chunked(q, k, v, log_a):
    b_, h_, s_, dk = q.shape
    dv = v.shape[-1]
    n = s_ // GLA_CHUNK
    q = q.reshape(b_, h_, n, GLA_CHUNK, dk)
    k = k.reshape(b_, h_, n, GLA_CHUNK, dk)
    v = v.reshape(b_, h_, n, GLA_CHUNK, dv)
    cum = jnp.cumsum(log_a.reshape(b_, h_, n, GLA_CHUNK, dk), axis=3)
    q_in = q * jnp.exp(cum)
    k_in = k * jnp.exp(-cum)
    causal = jnp.tril(jnp.ones((GLA_CHUNK, GLA_CHUNK), dtype=bool))
    attn = jnp.where(causal, jnp.einsum('bhncd,bhnjd->bhncj', q_in, k_in), 0.0)
    o_intra = jnp.einsum('bhncj,bhnjv->bhncv', attn, v)
    cum_last = cum[:, :, :, -1:, :]
    k_dec = k * jnp.exp(cum_last - cum)
    decay_chunk = jnp.exp(cum_last[:, :, :, 0, :])

    def step(state, inp):
        k_n, v_n, d_n = inp
        new_state = state * d_n[..., None] + jnp.einsum('bhcd,bhcv->bhdv', k_n, v_n)
        return new_state, state

    xs = (jnp.moveaxis(k_dec, 2, 0), jnp.moveaxis(v, 2, 0), jnp.moveaxis(decay_chunk, 2, 0))
    init = jnp.zeros((b_, h_, dk, dv), jnp.float32)
    _, states = lax.scan(step, init, xs)
    o_inter = jnp.einsum('bhncd,nbhdv->bhncv', q_in, states)
    return (o_intra + o_inter).reshape(b_, h_, s_, dv)


def gla_mixer(h, w_in, w_a2, b_a, o_gain, w_out):
    b_, s_, _ = h.shape
    proj = h @ w_in
    q, k, v, g, a_lr = jnp.split(
        proj, [GLA_KEY_DIM, 2 * GLA_KEY_DIM, 2 * GLA_KEY_DIM + GLA_VAL_DIM, 2 * GLA_KEY_DIM + 2 * GLA_VAL_DIM], axis=-1)
    log_a = jax.nn.log_sigmoid((a_lr @ w_a2).astype(jnp.float32) + b_a.astype(jnp.float32)) / GLA_GATE_NORMALIZER
    qh = to_heads(q, GLA_HEADS).astype(jnp.float32) * (GLA_DK ** -0.5)
    kh = to_heads(k, GLA_HEADS).astype(jnp.float32)
    vh = to_heads(v, GLA_HEADS).astype(jnp.float32)
    ah = to_heads(log_a, GLA_HEADS)
    o = gla_chunked(qh, kh, vh, ah).astype(h.dtype)
    o = rms_norm(o.transpose(0, 2, 1, 3), o_gain).reshape(b_, s_, GLA_VAL_DIM)
    return (o * jax.nn.silu(g)) @ w_out


def fox_attention(q, k, v, log_f):
    b_, h_, s_, d = q.shape
    nq = s_ // FOX_BLOCK
    c = jnp.cumsum(log_f, axis=-1)
    q_blocks = jnp.moveaxis(q.reshape(b_, h_, nq, FOX_BLOCK, d), 2, 0)
    c_blocks = jnp.moveaxis(c.reshape(b_, h_, nq, FOX_BLOCK), 2, 0)
    starts = jnp.arange(nq, dtype=jnp.int32) * FOX_BLOCK
    key_pos = jnp.arange(s_, dtype=jnp.int32)
    scale = FOX_HD ** -0.5

    def block(args):
        qb, cb, start = args
        q_pos = start + jnp.arange(FOX_BLOCK, dtype=jnp.int32)
        logits = (jnp.einsum('bhqd,bhkd->bhqk', qb, k).astype(jnp.float32) * scale
                  + cb[..., :, None] - c[..., None, :])
        mask = key_pos[None, :] <= q_pos[:, None]
        probs = jax.nn.softmax(jnp.where(mask, logits, -jnp.inf), axis=-1)
        return jnp.einsum('bhqk,bhkd->bhqd', probs.astype(v.dtype), v)

    out = lax.map(block, (q_blocks, c_blocks, starts))
    return jnp.moveaxis(out, 0, 2).reshape(b_, h_, s_, d)


def fox_mixer(h, w_in, b_f, q_gain, k_gain, w_out):
    b_, s_, _ = h.shape
    proj = h @ w_in
    q, k, v, g, f_logit = jnp.split(proj, [FOX_WIDTH, 2 * FOX_WIDTH, 3 * FOX_WIDTH, 4 * FOX_WIDTH], axis=-1)
    log_f = jax.nn.log_sigmoid(f_logit.astype(jnp.float32) + b_f.astype(jnp.float32))
    qh = rms_norm(to_heads(q, FOX_HEADS), q_gain)
    kh = rms_norm(to_heads(k, FOX_HEADS), k_gain)
    vh = to_heads(v, FOX_HEADS)
    o = fox_attention(qh, kh, vh, log_f.transpose(0, 2, 1))
    o = o.transpose(0, 2, 1, 3).reshape(b_, s_, FOX_WIDTH)
    return (o * jax.nn.silu(g)) @ w_out


def setup_inputs(seed: int = 0) -> dict:
    key = jax.random.key(seed)
    ks = jax.random.split(key, 20)
    f32 = jnp.float32
    nrm = lambda k, shape, fan_in: jax.random.normal(k, shape, f32) * (fan_in ** -0.5)
    gain = lambda k, shape: 1.0 + 0.02 * jax.random.normal(k, shape, f32)
    return {
        "x": jax.random.normal(ks[0], (BATCH, SEQ, D_MODEL), f32),
        "p": jax.random.normal(ks[1], (DEPTH, BATCH, SEQ, PLE_DIM), f32),
        "norm_mixer": gain(ks[2], (DEPTH, D_MODEL)),
        "gla_w_in": nrm(ks[3], (N_GLA, D_MODEL, GLA_IN), D_MODEL),
        "gla_w_a2": nrm(ks[4], (N_GLA, GLA_GATE_RANK, GLA_KEY_DIM), GLA_GATE_RANK),
        "gla_b_a": 0.1 * jax.random.normal(ks[5], (N_GLA, GLA_KEY_DIM), f32),
        "gla_o_norm": gain(ks[6], (N_GLA, GLA_DV)),
        "gla_w_out": nrm(ks[7], (N_GLA, GLA_VAL_DIM, D_MODEL), GLA_VAL_DIM),
        "fox_w_in": nrm(ks[8], (N_FOX, D_MODEL, FOX_IN), D_MODEL),
        "fox_b_f": 2.0 + 0.5 * jax.random.normal(ks[9], (N_FOX, FOX_HEADS), f32),
        "fox_q_norm": gain(ks[10], (N_FOX, FOX_HD)),
        "fox_k_norm": gain(ks[11], (N_FOX, FOX_HD)),
        "fox_w_out": nrm(ks[12], (N_FOX, FOX_WIDTH, D_MODEL), FOX_WIDTH),
        "ple_proj": nrm(ks[13], (DEPTH, PLE_DIM, D_MODEL), PLE_DIM),
        "ple_gate": nrm(ks[14], (DEPTH, D_MODEL, D_MODEL), D_MODEL),
        "ple_norm": gain(ks[15], (DEPTH, D_MODEL)),
        "final_norm": gain(ks[16], (D_MODEL,)),
    }


def reference(x, p, norm_mixer, gla_w_in, gla_w_a2, gla_b_a, gla_o_norm, gla_w_out,
              fox_w_in, fox_b_f, fox_q_norm, fox_k_norm, fox_w_out,
              ple_proj, ple_gate, ple_norm, final_norm):
    h = x
    for i in range(DEPTH):
        hn = rms_norm(h, norm_mixer[i])
        j = i // N_MIXERS
        if i % N_MIXERS == 0:
            h = h + gla_mixer(hn, gla_w_in[j], gla_w_a2[j], gla_b_a[j], gla_o_norm[j], gla_w_out[j])
        else:
            h = h + fox_mixer(hn, fox_w_in[j], fox_b_f[j], fox_q_norm[j], fox_k_norm[j], fox_w_out[j])
        h = h + (p[i] @ ple_proj[i]) * jax.nn.sigmoid(rms_norm(h, ple_norm[i]) @ ple_gate[i])
    return rms_norm(h, final_norm)
```

```python
import numpy as np
from contextlib import ExitStack
import concourse.bass as bass
import concourse.mybir as mybir
from concourse.bass_utils import run_bass_kernel_spmd

F32 = mybir.dt.float32
BF16 = mybir.dt.bfloat16
AF = mybir.ActivationFunctionType
ALU = mybir.AluOpType
AX = mybir.AxisListType

D = 1024
SEQ = 2048
NB = SEQ // 128
EPS = 1e-6


class Tok:
    __slots__ = ("sem", "val", "eng", "dma", "seq")


class Buf:
    def __init__(self, name):
        self.name = name
        self.w = None
        self.r = {}


class TK:
    EPOCH = 12000

    def __init__(self, nc, st):
        self.nc, self.st = nc, st
        self.prog = {e: [] for e in ("pe", "act", "dve", "pool", "sp")}
        self.sems, self.cnt, self.waited = {}, {}, {}
        self.pending = {e: [] for e in self.prog}
        self.nsem = 0
        self.seq = 0
        self.nins = {e: 0 for e in self.prog}
        for e in ("pe", "act", "dve"):
            self._new_epoch(e)

    def new_sem(self, name):
        self.nsem += 1
        return self.st.enter_context(self.nc.semaphore(f"{name}_{self.nsem}"))

    def new_dma(self, name):
        return (self.new_sem(name), {"val": 0})

    def _new_epoch(self, e):
        self.sems[e] = self.new_sem("s_" + e)
        self.cnt[e] = 0

    def op(self, eng, fn, reads=(), writes=(), sig=True, dma=None):
        deps = []
        for b in reads:
            if b.w is not None:
                deps.append(b.w)
        for b in writes:
            if b.w is not None:
                deps.append(b.w)
            for t in b.r.values():
                if (not t.dma) and t.eng == eng:
                    continue
                deps.append(t)
        prog = self.prog[eng]
        for d in deps:
            if (not d.dma) and d.eng == eng and eng == "pe":
                continue
            if d.val is None:
                raise RuntimeError("unresolved dependency token")
            key = (eng, id(d.sem))
            if self.waited.get(key, -1) >= d.val:
                continue
            self.waited[key] = d.val
            prog.append(lambda e, s=d.sem, v=d.val: e.wait_ge(s, v))
        tok = Tok()
        tok.eng = eng
        tok.dma = dma is not None
        self.seq += 1
        tok.seq = self.seq
        self.nins[eng] += 1
        if dma is not None:
            sem, stt = dma
            stt["val"] += 16
            tok.sem, tok.val = sem, stt["val"]
            prog.append(lambda e, f=fn, s=sem: f(e).then_inc(s, 16))
        elif sig:
            if self.cnt[eng] >= self.EPOCH:
                self._new_epoch(eng)
            self.cnt[eng] += 1
            tok.sem, tok.val = self.sems[eng], self.cnt[eng]
            for t in self.pending[eng]:
                t.sem, t.val = tok.sem, tok.val
            self.pending[eng] = []
            prog.append(lambda e, f=fn, s=tok.sem: f(e).then_inc(s, 1))
        else:
            tok.sem, tok.val = None, None
            self.pending[eng].append(tok)
            prog.append(lambda e, f=fn: f(e))
        for b in reads:
            b.r[id(tok.sem) if tok.dma else eng] = tok
        for b in writes:
            b.w = tok
            b.r = {}
        return tok

    def alias(self, old, new):
        merged = {}
        for ob in old:
            for t in ([ob.w] if ob.w is not None else []) + list(ob.r.values()):
                k = id(t.sem) if t.dma else t.eng
                if k not in merged or merged[k].seq < t.seq:
                    merged[k] = t
        for nb in new:
            nb.w = None
            nb.r = dict(merged)


class T:
    def __init__(self, ap, name):
        self.ap = ap
        self.b = Buf(name)


def build(nseq=2, layers=(0, 1, 2, 3), final=True):
    nc = bass.Bass("TRN2", target_bir_lowering=False)
    dt_in = lambda name, shape: nc.dram_tensor(name, list(shape), F32, kind="ExternalInput").ap()
    x = dt_in("x", [nseq, SEQ, D])
    p = dt_in("p", [4, nseq, SEQ, 256])
    norm_mixer = dt_in("norm_mixer", [4, D])
    gla_w_in = dt_in("gla_w_in", [2, D, 3088])
    gla_w_a2 = dt_in("gla_w_a2", [2, 16, 512])
    gla_b_a = dt_in("gla_b_a", [2, 512])
    gla_o_norm = dt_in("gla_o_norm", [2, 256])
    gla_w_out = dt_in("gla_w_out", [2, D, D])
    fox_w_in = dt_in("fox_w_in", [2, D, 4112])
    fox_b_f = dt_in("fox_b_f", [2, 16])
    fox_q_norm = dt_in("fox_q_norm", [2, 64])
    fox_k_norm = dt_in("fox_k_norm", [2, 64])
    fox_w_out = dt_in("fox_w_out", [2, D, D])
    ple_proj = dt_in("ple_proj", [4, 256, D])
    ple_gate = dt_in("ple_gate", [4, D, D])
    ple_norm = dt_in("ple_norm", [4, D])
    final_norm = dt_in("final_norm", [1, D])
    out = nc.dram_tensor("out", [nseq, SEQ, D], F32, kind="ExternalOutput").ap()

    with ExitStack() as st:
        tk = TK(nc, st)

        def sb(name, shape, dt):
            return st.enter_context(nc.sbuf_tensor(name, list(shape), dt))

        def sbt(name, shape, dt):
            return T(sb(name, shape, dt), name)

        h = sb("h", [128, NB, D], F32)
        hB = [Buf(f"h{b}") for b in range(NB)]
        arena = sb("arena", [128, 33792], BF16)
        arena2 = sb("arena2", [128, 16384], BF16)
        gla_Win = T(arena[:, 0:8 * 3088].rearrange("p (k c) -> p k c", k=8), "gla_Win")
        Wout = T(arena[:, 25600:25600 + 8192].rearrange("p (k c) -> p k c", k=8), "Wout")
        fox_Wg = T(arena[:, 0:8192].rearrange("p (k j c) -> p k j c", k=8, j=4), "fox_Wg")
        fox_KT = T(arena[:, 8192:16384].rearrange("p (h t) -> p h t", h=4), "fox_KT")
        fox_V = T(arena[:, 16384:16384 + 16 * 4 * 65].rearrange("p (b h c) -> p b h c", b=16, h=4), "fox_V")
        fox_Wf = T(arena[:, 20736:20736 + 128].rearrange("p (k c) -> p k c", k=8), "fox_Wf")
        ple_Wg = T(arena[:, 0:8192].rearrange("p (k c) -> p k c", k=8), "ple_Wg")
        ple_Wp = T(arena[:, 8192:10240].rearrange("p (k c) -> p k c", k=2), "ple_Wp")
        KTb = [Buf(f"KT{b}") for b in range(NB)]
        Vb = [Buf(f"V{b}") for b in range(NB)]
        arena_gla = [gla_Win.b]
        arena_fox = [fox_Wg.b, fox_Wf.b] + KTb + Vb
        arena_ple = [ple_Wg.b, ple_Wp.b]
        hT = arena2[:, :].rearrange("p (k t) -> p k t", k=8)
        hTB = [Buf(f"hT{b}") for b in range(NB)]
        a2 = [0]

        def carve(name, n_bf16, dt=BF16):
            ap_ = arena2[:, a2[0]:a2[0] + n_bf16]
            a2[0] += n_bf16
            if dt == F32:
                ap_ = ap_.bitcast(F32)
            return T(ap_, name)

        xe = carve("xe", 1024, F32)
        Lbf = carve("Lbf", 512)
        E_tm = carve("E_tm", 1024, F32)
        Einv_tm = carve("Einv_tm", 1024, F32)
        q_in = carve("q_in", 512)
        k_in = carve("k_in", 512)
        q_inT = carve("q_inT", 512)
        k_inT = carve("k_inT", 512)
        v_sb = carve("v_sb", 1024)
        attnT = carve("attnT", 512)
        S_f32 = carve("S_f32", 2048, F32)
        S_bf = carve("S_bf", 1024)
        gla_work = [xe, Lbf, E_tm, Einv_tm, q_in, k_in, q_inT, k_inT, v_sb, attnT, S_f32, S_bf]
        assert a2[0] <= 16384

        gbc_m = sbt("gbc_m", [128, D], F32)
        gbc_p = sbt("gbc_p", [128, D], F32)
        hb = [sbt(f"hb{i}", [128, D], BF16) for i in range(2)]
        sqj = sbt("sqj", [128, D], BF16)
        hTb = [sbt(f"hTb{i}", [128, 8, 128], BF16) for i in range(2)]
        e_g = sbt("e_g", [128, D], F32)
        sg = sbt("sg", [128, D], F32)
        og = sbt("og", [128, D], BF16)
        ogT = sbt("ogT", [128, 8, 128], BF16)
        p_b = [sbt(f"p_b{i}", [128, 256], BF16) for i in range(2)]
        pT = sbt("pT", [128, 2, 128], BF16)
        stats = sbt("stats", [128, 64], F32)
        rstd_all = sbt("rstd_all", [128, NB], F32)
        ident = sbt("ident", [128, 128], BF16)
        maskT = sbt("maskT", [128, 128], BF16)
        tri32 = sbt("tri32", [128, 128], F32)
        ones32 = sbt("ones32", [128, 128], F32)
        ones_bf = sbt("ones_bf", [128, 1], BF16)
        cf = sbt("cf", [128, 128], F32)
        a_sb = sbt("a_sb", [128, 16], BF16)
        aT = sbt("aT", [17, 128], BF16)
        wa2 = sbt("wa2", [17, 512], BF16)
        gcol = sbt("gcol", [128, 2], F32)
        decay = sbt("decay", [128, 4], F32)
        sso = sbt("sso", [128, 8], F32)
        kq_aug = [sbt(f"kq_aug{i}", [128, 4, 68], BF16) for i in range(2)]
        qT = sbt("qT", [68, 4, 128], BF16)
        PT = [sbt(f"PT{i}", [128, 4, 128], BF16) for i in range(2)]
        sqk = sbt("sqk", [128, 256], F32)
        fst = sbt("fst", [128, 64], F32)
        kgain = sbt("kgain", [128, 64], F32)
        qgain = sbt("qgain", [128, 64], F32)
        bf_bc = sbt("bf_bc", [128, 16], F32)
        Lf = sbt("Lf", [128, 16], F32)
        Rf = sbt("Rf", [128, 16], F32)
        C_all = sbt("C_all", [128, NB, 16], F32)
        Chl = sbt("Chl", [128, NB, 2, 16], BF16)
        Ctmp = sbt("Ctmp", [128, 16], F32)
        CB = [Buf(f"C{b}") for b in range(NB)]
        rden = sbt("rden", [128, 4], F32)
        otmp = sbt("otmp", [128, 256], F32)

        psF = [T(st.enter_context(nc.psum_tensor(f"psF{i}", [128, 512], F32)), f"psF{i}") for i in range(6)]
        psB = [T(st.enter_context(nc.psum_tensor(f"psB{i}", [128, 1024], BF16)), f"psB{i}") for i in range(2)]
        rr = {"F": 0, "B": 0, "hb": 0, "hTb": 0, "pb": 0, "PT": 0, "aug": 0}

        def nextF(n=5):
            rr["F"] = (rr["F"] + 1) % n
            return psF[rr["F"]]

        def nextB():
            rr["B"] ^= 1
            return psB[rr["B"]]

        def rot(key, lst):
            rr[key] = (rr[key] + 1) % len(lst)
            return lst[rr[key]]

        def bl(xs):
            return [t.b if isinstance(t, T) else t for t in xs]

        def mm(o, lhsT, rhs, start, stop, reads, writes, sig=None):
            tk.op("pe", lambda e: e.matmul(o, lhsT=lhsT, rhs=rhs, start=start, stop=stop),
                  bl(reads), bl(writes), sig=(stop if sig is None else sig))

        def tr(o, i, reads, writes, sig=True):
            tk.op("pe", lambda e: e.transpose(out=o, in_=i, identity=ident.ap[:]), bl(reads) + [ident.b], bl(writes), sig=sig)

        def act(o, i, func, reads, writes, bias=None, scale=None, accum=None):
            kw = {}
            if bias is not None:
                kw["bias"] = bias
            if scale is not None:
                kw["scale"] = scale
            if accum is not None:
                kw["accum_out"] = accum
            tk.op("act", lambda e: e.activation(out=o, in_=i, func=func, **kw), bl(reads), bl(writes))

        def dve(fn, reads, writes):
            tk.op("dve", fn, bl(reads), bl(writes))

        def tt(o, a, b_, op, reads, writes):
            dve(lambda e: e.tensor_tensor(out=o, in0=a, in1=b_, op=op), reads, writes)

        def ts(o, a, s1, s2, op0, op1, reads, writes):
            if s2 is None:
                dve(lambda e: e.tensor_scalar(out=o, in0=a, scalar1=s1, scalar2=None, op0=op0), reads, writes)
            else:
                dve(lambda e: e.tensor_scalar(out=o, in0=a, scalar1=s1, scalar2=s2, op0=op0, op1=op1), reads, writes)

        def stt(o, a, s, b_, op0, op1, reads, writes):
            dve(lambda e: e.scalar_tensor_tensor(out=o, in0=a, scalar=s, in1=b_, op0=op0, op1=op1), reads, writes)

        def cp(o, i, reads, writes):
            dve(lambda e: e.tensor_copy(out=o, in_=i), reads, writes)

        def wload(o, i, writes, dmah):
            tk.op("pool", lambda e: e.dma_start(out=o, in_=i), [], bl(writes), dma=dmah)

        def wload_nc(o, i, writes, dmah):
            tk.op("pool", lambda e: e.dma_start(out=o, in_=i, allow_slow_non_contiguous=True), [], bl(writes), dma=dmah)

        dm = {}

        def dmah(name):
            if name not in dm:
                dm[name] = tk.new_dma(name)
            return dm[name]

        pl = lambda fn, reads, writes: tk.op("dve", fn, bl(reads), bl(writes))
        tk._new_epoch("pool")

        def pool(fn, reads, writes):
            tk.op("pool", fn, bl(reads), bl(writes))

        pool(lambda e: e.memset(ones32.ap[:], 1.0), [], [ones32])
        pool(lambda e: e.memset(tri32.ap[:], 1.0), [], [tri32])
        pool(lambda e: e.affine_select(out=tri32.ap[:], in_=tri32.ap[:], pattern=[[1, 128]], compare_op=ALU.is_ge,
                                       fill=0.0, base=0, channel_multiplier=-1), [tri32], [tri32])
        pool(lambda e: e.memset(cf.ap[:], 1.0), [], [cf])
        pool(lambda e: e.affine_select(out=cf.ap[:], in_=cf.ap[:], pattern=[[-1, 128]], compare_op=ALU.is_equal,
                                       fill=0.0, base=0, channel_multiplier=1), [cf], [cf])
        cp(ident.ap[:], cf.ap[:], [cf], [ident])
        cp(maskT.ap[:], tri32.ap[:], [tri32], [maskT])
        cp(ones_bf.ap[:], ones32.ap[:, 0:1], [ones32], [ones_bf])
        dve(lambda e: e.memset(aT.ap[:], 1.0), [], [aT])

        def pass0(b, gbc, rstd_ap, hT_dst, hT_buf):
            hbt = rot("hb", hb)
            act(sqj.ap[:], h[:, b, :], AF.Square, [hB[b]], [sqj, stats], accum=stats.ap[:, 0:1])
            act(stats.ap[:, 1:2], stats.ap[:, 0:1], AF.Ln, [stats], [stats], bias=EPS, scale=1.0 / D)
            act(rstd_ap, stats.ap[:, 1:2], AF.Exp, [stats], [rstd_all], scale=-0.5)
            tt(hbt.ap[:], h[:, b, :], gbc.ap[:], ALU.mult, [hB[b], gbc], [hbt])
            pb_ = nextB()
            for k in range(8):
                tr(pb_.ap[:, k * 128:(k + 1) * 128], hbt.ap[:, k * 128:(k + 1) * 128], [hbt], [pb_], sig=(k == 7))
            cp(hT_dst, pb_.ap[:, :].rearrange("p (k t) -> p k t", k=8), [pb_], [hT_buf])

        def load_gbc(dst, src_row):
            wload(dst.ap[:], src_row.to_broadcast([128, D]), [dst], dmah(dst.b.name))

        def proj(hT_ap, hT_buf, W_ap_fn, ncols, Wbuf, bank):
            for k in range(8):
                mm(bank.ap[:, 0:ncols], hT_ap[:, k, :], W_ap_fn(k), k == 0, k == 7, [hT_buf, Wbuf], [bank])

        def ple_pass(i, s):
            tk.alias(arena_gla + arena_fox, arena_ple)
            wg_view = ple_gate[i].rearrange("(k p) c -> p k c", p=128)
            for k in range(8):
                wload(ple_Wg.ap[:, k, :], wg_view[:, k, :], [ple_Wg], dmah("ple_Wg"))
            wload(ple_Wp.ap[:, :, :], ple_proj[i].rearrange("(k p) c -> p k c", p=128), [ple_Wp], dmah("ple_Wp"))
            load_gbc(gbc_p, ple_norm[i:i + 1, :])
            for b in range(NB):
                rs = rstd_all.ap[:, b:b + 1]
                hTt = rot("hTb", hTb)
                pbt = rot("pb", p_b)
                wload(pbt.ap[:], p[i, s, b * 128:(b + 1) * 128, :], [pbt], dmah(pbt.b.name))
                pass0(b, gbc_p, rs, hTt.ap[:], hTt.b)
                pb_ = nextB()
                for k in range(2):
                    tr(pb_.ap[:, k * 128:(k + 1) * 128], pbt.ap[:, k * 128:(k + 1) * 128], [pbt], [pb_], sig=(k == 1))
                cp(pT.ap[:], pb_.ap[:, 0:256].rearrange("p (k t) -> p k t", k=2), [pb_], [pT])
                for cg in range(2):
                    cs = slice(cg * 512, (cg + 1) * 512)
                    bg = nextF()
                    proj(hTt.ap, hTt.b, lambda k: ple_Wg.ap[:, k, cs], 512, ple_Wg.b, bg)
                    ts(stats.ap[:, 2:3], rs, -1.0, None, ALU.mult, None, [rstd_all], [stats]) if cg == 0 else None
                    act(e_g.ap[:, cs], bg.ap[:], AF.Exp, [bg, stats], [e_g], scale=stats.ap[:, 2:3])
                    ts(e_g.ap[:, cs], e_g.ap[:, cs], 1.0, None, ALU.add, None, [e_g], [e_g])
                    dve(lambda e, cs=cs: e.reciprocal(out=sg.ap[:, cs], in_=e_g.ap[:, cs]), [e_g], [sg])
                    bp = nextF()
                    for k in range(2):
                        mm(bp.ap[:], pT.ap[:, k, :], ple_Wp.ap[:, k, cs], k == 0, k == 1, [pT, ple_Wp], [bp])
                    tt(sg.ap[:, cs], bp.ap[:], sg.ap[:, cs], ALU.mult, [bp, sg], [sg])
                    tt(h[:, b, cs], h[:, b, cs], sg.ap[:, cs], ALU.add, [hB[b], sg], [hB[b]])

        def gla_layer(i, s):
            j = i // 2
            tk.alias(arena_ple + arena_fox, arena_gla)
            tk.alias(hTB, [t.b for t in gla_work])
            wv = gla_w_in[j].rearrange("(k p) c -> p k c", p=128)
            for (c0, c1) in ((3072, 3088), (0, 512), (512, 1024), (1024, 1536), (1536, 2048), (2048, 2560), (2560, 3072)):
                for k0 in (0, 4):
                    wload(gla_Win.ap[:, k0:k0 + 4, c0:c1], wv[:, k0:k0 + 4, c0:c1], [gla_Win], dmah("gla_Win"))
            wo = gla_w_out[j].rearrange("(k p) c -> p k c", p=128)
            for k0 in (0, 4):
                wload(Wout.ap[:, k0:k0 + 4, :], wo[:, k0:k0 + 4, :], [Wout], dmah("Wout"))
            wload(wa2.ap[0:16, :], gla_w_a2[j], [wa2], dmah("wa2"))
            wload(wa2.ap[16:17, :], gla_b_a[j:j + 1, :], [wa2], dmah("wa2"))
            tk.op("sp", lambda e: e.dma_start(out=gcol.ap[:], in_=gla_o_norm[j].rearrange("(a p) -> p a", p=128),
                                              allow_slow_non_contiguous=True), [], [gcol.b], dma=dmah("gcol"))
            load_gbc(gbc_m, norm_mixer[i:i + 1, :])
            for k in range(8):
                ts(Wout.ap[:, k, :], Wout.ap[:, k, :], gcol.ap[:, (k % 2):(k % 2) + 1], None, ALU.mult, None, [Wout, gcol], [Wout])
            Win = gla_Win
            for b in range(NB):
                rs = rstd_all.ap[:, b:b + 1]
                hTt = rot("hTb", hTb)
                pass0(b, gbc_m, rs, hTt.ap[:], hTt.b)
                ba = nextF()
                proj(hTt.ap, hTt.b, lambda k: Win.ap[:, k, 3072:3088], 16, Win.b, ba)
                act(a_sb.ap[:], ba.ap[:, 0:16], AF.Copy, [ba, rstd_all], [a_sb], scale=rs)
                pb_ = nextB()
                tr(pb_.ap[0:16, 0:128], a_sb.ap[:], [a_sb], [pb_])
                cp(aT.ap[0:16, :], pb_.ap[0:16, 0:128], [pb_], [aT])
                bx = nextF()
                mm(bx.ap[:], aT.ap[:], wa2.ap[:], True, True, [aT, wa2], [bx])
                act(xe.ap[:], bx.ap[:], AF.Exp, [bx], [xe], scale=-1.0)
                act(xe.ap[:], xe.ap[:], AF.Ln, [xe], [xe], bias=1.0)
                cp(Lbf.ap[:], xe.ap[:], [xe], [Lbf])
                bc = nextF()
                mm(bc.ap[:], maskT.ap[:], Lbf.ap[:], True, True, [maskT, Lbf], [bc])
                act(E_tm.ap[:], bc.ap[:], AF.Exp, [bc], [E_tm], scale=-1.0 / 16, bias=float(np.log(128.0 ** -0.5)))
                act(Einv_tm.ap[:], bc.ap[:], AF.Exp, [bc], [Einv_tm], scale=1.0 / 16)
                bd = nextF()
                for hh in range(4):
                    mm(bd.ap[:, hh:hh + 1], Lbf.ap[:, hh * 128:(hh + 1) * 128], ones_bf.ap[:], True, True, [Lbf, ones_bf], [bd], sig=(hh == 3))
                act(decay.ap[:], bd.ap[:, 0:4], AF.Exp, [bd], [decay], scale=-1.0 / 16)
                bq = nextF()
                proj(hTt.ap, hTt.b, lambda k: Win.ap[:, k, 0:512], 512, Win.b, bq)
                stt(q_in.ap[:], bq.ap[:], rs, E_tm.ap[:], ALU.mult, ALU.mult, [bq, rstd_all, E_tm], [q_in])
                bk_ = nextF()
                proj(hTt.ap, hTt.b, lambda k: Win.ap[:, k, 512:1024], 512, Win.b, bk_)
                stt(k_in.ap[:], bk_.ap[:], rs, Einv_tm.ap[:], ALU.mult, ALU.mult, [bk_, rstd_all, Einv_tm], [k_in])
                for (src, dst) in ((q_in, q_inT), (k_in, k_inT)):
                    pb_ = nextB()
                    for hh in range(4):
                        tr(pb_.ap[:, hh * 128:(hh + 1) * 128], src.ap[:, hh * 128:(hh + 1) * 128], [src], [pb_], sig=(hh == 3))
                    cp(dst.ap[:], pb_.ap[:, 0:512], [pb_], [dst])
                for cg in range(2):
                    bv = nextF()
                    proj(hTt.ap, hTt.b, lambda k: Win.ap[:, k, 1024 + cg * 512:1536 + cg * 512], 512, Win.b, bv)
                    act(v_sb.ap[:, cg * 512:(cg + 1) * 512], bv.ap[:], AF.Copy, [bv, rstd_all], [v_sb], scale=rs)
                bt = nextF()
                for hh in range(4):
                    mm(bt.ap[:, hh * 128:(hh + 1) * 128], k_inT.ap[:, hh * 128:(hh + 1) * 128], q_inT.ap[:, hh * 128:(hh + 1) * 128],
                       True, True, [k_inT, q_inT], [bt], sig=(hh == 3))
                tt(attnT.ap[:].rearrange("p (h c) -> p h c", h=4), bt.ap[:].rearrange("p (h c) -> p h c", h=4),
                   maskT.ap[:].unsqueeze(1).to_broadcast([128, 4, 128]), ALU.mult, [bt, maskT], [attnT])
                bo = [nextF(), nextF()]
                for hh in range(4):
                    o_ap = bo[hh // 2].ap[:, (hh % 2) * 256:(hh % 2) * 256 + 256]
                    mm(o_ap, attnT.ap[:, hh * 128:(hh + 1) * 128], v_sb.ap[:, hh * 256:(hh + 1) * 256], True, b == 0,
                       [attnT, v_sb], [bo[hh // 2]], sig=(b == 0 and hh % 2 == 1))
                    if b > 0:
                        mm(o_ap, q_inT.ap[:, hh * 128:(hh + 1) * 128], S_bf.ap[:, hh * 256:(hh + 1) * 256], False, True,
                           [q_inT, S_bf], [bo[hh // 2]], sig=(hh % 2 == 1))
                for hh in range(4):
                    o_ap = bo[hh // 2].ap[:, (hh % 2) * 256:(hh % 2) * 256 + 256]
                    act(sqj.ap[:, 0:256], o_ap, AF.Square, [bo[hh // 2]], [sqj, sso], accum=sso.ap[:, hh:hh + 1])
                act(sso.ap[:, 4:8], sso.ap[:, 0:4], AF.Ln, [sso], [sso], bias=EPS, scale=1.0 / 256)
                act(sso.ap[:, 0:4], sso.ap[:, 4:8], AF.Exp, [sso], [sso], scale=-0.5)
                ts(stats.ap[:, 2:3], rs, -1.0, None, ALU.mult, None, [rstd_all], [stats])
                bgs = []
                for cg in range(2):
                    cs = slice(cg * 512, (cg + 1) * 512)
                    bg = nextF()
                    bgs.append(bg)
                    proj(hTt.ap, hTt.b, lambda k: Win.ap[:, k, 2048 + cg * 512:2560 + cg * 512], 512, Win.b, bg)
                    act(e_g.ap[:, cs], bg.ap[:], AF.Exp, [bg, stats], [e_g], scale=stats.ap[:, 2:3])
                    ts(e_g.ap[:, cs], e_g.ap[:, cs], 1.0, None, ALU.add, None, [e_g], [e_g])
                    dve(lambda e, cs=cs: e.reciprocal(out=e_g.ap[:, cs], in_=e_g.ap[:, cs]), [e_g], [e_g])
                    stt(sg.ap[:, cs], bg.ap[:], rs, e_g.ap[:, cs], ALU.mult, ALU.mult, [bg, rstd_all, e_g], [sg])
                for hh in range(4):
                    o_ap = bo[hh // 2].ap[:, (hh % 2) * 256:(hh % 2) * 256 + 256]
                    stt(og.ap[:, hh * 256:(hh + 1) * 256], o_ap, sso.ap[:, hh:hh + 1], sg.ap[:, hh * 256:(hh + 1) * 256],
                        ALU.mult, ALU.mult, [bo[hh // 2], sso, sg], [og])
                if b < NB - 1:
                    for half in range(2):
                        bs = nextF()
                        for hq in range(2):
                            hh = half * 2 + hq
                            mm(bs.ap[:, hq * 256:(hq + 1) * 256], k_in.ap[:, hh * 128:(hh + 1) * 128], v_sb.ap[:, hh * 256:(hh + 1) * 256],
                               True, True, [k_in, v_sb], [bs], sig=(hq == 1))
                        Sv = S_f32.ap[:, half * 512:(half + 1) * 512]
                        if b == 0:
                            cp(Sv, bs.ap[:], [bs], [S_f32])
                        else:
                            tt(Sv, Sv, bs.ap[:], ALU.add, [bs, S_f32], [S_f32])
                        for hq in range(2):
                            hh = half * 2 + hq
                            Sh = S_f32.ap[:, hh * 256:(hh + 1) * 256]
                            ts(Sh, Sh, decay.ap[:, hh:hh + 1], None, ALU.mult, None, [S_f32, decay], [S_f32])
                    act(S_bf.ap[:], S_f32.ap[:], AF.Copy, [S_f32], [S_bf])
                out_proj(b, range(8))

        def out_proj(b, chunks):
            chunks = list(chunks)
            n = len(chunks)
            pb_ = nextB()
            for ci, k in enumerate(chunks):
                tr(pb_.ap[:, ci * 128:(ci + 1) * 128], og.ap[:, ci * 128:(ci + 1) * 128], [og], [pb_], sig=(ci == n - 1))
            dve(lambda e: e.tensor_copy(out=ogT.ap[:, 0:n, :], in_=pb_.ap[:, 0:n * 128].rearrange("p (k t) -> p k t", k=n)), [pb_], [ogT])
            for cg in range(2):
                cs = slice(cg * 512, (cg + 1) * 512)
                by = nextF()
                for ci, k in enumerate(chunks):
                    mm(by.ap[:], ogT.ap[:, ci, :], Wout.ap[:, k, cs], ci == 0, ci == n - 1, [ogT, Wout], [by])
                tt(h[:, b, cs], h[:, b, cs], by.ap[:], ALU.add, [hB[b], by], [hB[b]])

        def fox_layer(i, s):
            j = i // 2
            tk.alias(arena_ple + arena_gla, arena_fox)
            tk.alias([t.b for t in gla_work], hTB)
            wv = fox_w_in[j].rearrange("(k p) c -> p k c", p=128)
            wo = fox_w_out[j].rearrange("(k p) c -> p k c", p=128)
            for k0 in (0, 4):
                wload(Wout.ap[:, k0:k0 + 4, :], wo[:, k0:k0 + 4, :], [Wout], dmah("Wout"))
            wload(fox_Wf.ap[:, :, :], wv[:, :, 4096:4112], [fox_Wf], dmah("fox_Wf"))
            load_gbc(gbc_m, norm_mixer[i:i + 1, :])
            wload(kgain.ap[:], fox_k_norm[j:j + 1, :].to_broadcast([128, 64]), [kgain], dmah("kgain"))
            wload(qgain.ap[:], fox_q_norm[j:j + 1, :].to_broadcast([128, 64]), [qgain], dmah("qgain"))
            wload(bf_bc.ap[:], fox_b_f[j:j + 1, :].to_broadcast([128, 16]), [bf_bc], dmah("bf_bc"))
            ts(qgain.ap[:], qgain.ap[:], 0.125, None, ALU.mult, None, [qgain], [qgain])
            dve(lambda e: e.memset(Rf.ap[:], 0.0), [], [Rf])
            for b in range(NB):
                rs = rstd_all.ap[:, b:b + 1]
                pass0(b, gbc_m, rs, hT[:, :, b * 128:(b + 1) * 128], hTB[b])
                bf_ = nextF()
                proj(hT[:, :, b * 128:(b + 1) * 128], hTB[b], lambda k: fox_Wf.ap[:, k, :], 16, fox_Wf.b, bf_)
                stt(Lf.ap[:], bf_.ap[:, 0:16], rs, bf_bc.ap[:], ALU.mult, ALU.add, [bf_, rstd_all, bf_bc], [Lf])
                act(Lf.ap[:], Lf.ap[:], AF.Exp, [Lf], [Lf], scale=-1.0)
                act(Lf.ap[:], Lf.ap[:], AF.Ln, [Lf], [Lf], bias=1.0)
                bc = nextF()
                mm(bc.ap[:, 0:16], tri32.ap[:], Lf.ap[:], True, False, [tri32, Lf], [bc], sig=False)
                mm(bc.ap[:, 0:16], ones32.ap[:], Rf.ap[:], False, True, [ones32, Rf], [bc])
                cp(C_all.ap[:, b, :], bc.ap[:, 0:16], [bc], [CB[b]])
                tt(Rf.ap[:], Rf.ap[:], Lf.ap[:], ALU.add, [Rf, Lf], [Rf])
                cp(Chl.ap[:, b, 0, :], C_all.ap[:, b, :], [CB[b]], [CB[b]])
                tt(Ctmp.ap[:], C_all.ap[:, b, :], Chl.ap[:, b, 0, :], ALU.subtract, [CB[b]], [Ctmp])
                cp(Chl.ap[:, b, 1, :], Ctmp.ap[:], [Ctmp], [CB[b]])
            dve(lambda e: e.memset(fox_V.ap[:, :, :, 64:65], 1.0), [], Vb)

            def qk_norm_aug(bank, rs, b, G, gain, is_q):
                aug = rot("aug", kq_aug)
                act(sqk.ap[:], bank.ap[:, 0:256], AF.Square, [bank], [sqk])
                dve(lambda e: e.reduce_sum(out=fst.ap[:, 0:4], in_=sqk.ap[:].rearrange("p (h d) -> p h d", h=4), axis=AX.X), [sqk], [fst])
                tt(fst.ap[:, 8:9], rs, rs, ALU.mult, [rstd_all], [fst])
                ts(fst.ap[:, 4:8], fst.ap[:, 0:4], fst.ap[:, 8:9], None, ALU.mult, None, [fst], [fst])
                act(fst.ap[:, 12:16], fst.ap[:, 4:8], AF.Ln, [fst], [fst], bias=EPS, scale=1.0 / 64)
                act(fst.ap[:, 16:20], fst.ap[:, 12:16], AF.Exp, [fst], [fst], scale=-0.5)
                ts(fst.ap[:, 20:24], fst.ap[:, 16:20], rs, None, ALU.mult, None, [fst, rstd_all], [fst])
                tt(sqk.ap[:].rearrange("p (h d) -> p h d", h=4), bank.ap[:, 0:256].rearrange("p (h d) -> p h d", h=4),
                   fst.ap[:, 20:24].unsqueeze(2).to_broadcast([128, 4, 64]), ALU.mult, [bank, fst], [sqk])
                tt(aug.ap[:, :, 0:64], sqk.ap[:].rearrange("p (h d) -> p h d", h=4),
                   gain.ap[:].unsqueeze(1).to_broadcast([128, 4, 64]), ALU.mult, [sqk, gain], [aug])
                hs = slice(G * 4, G * 4 + 4)
                if is_q:
                    ts(aug.ap[:, :, 64:66], Chl.ap[:, b, :, hs].rearrange("p a h -> p h a"), -1.0, None, ALU.mult, None, [CB[b]], [aug])
                    dve(lambda e: e.memset(aug.ap[:, :, 66:68], 1.0), [], [aug])
                else:
                    dve(lambda e: e.memset(aug.ap[:, :, 64:66], 1.0), [], [aug])
                    cp(aug.ap[:, :, 66:68], Chl.ap[:, b, :, hs].rearrange("p a h -> p h a"), [CB[b]], [aug])
                return aug

            for G in range(4):
                for jj in range(4):
                    c0 = jj * 1024 + G * 256
                    wload(fox_Wg.ap[:, :, jj, :], wv[:, :, c0:c0 + 256], [fox_Wg], dmah("fox_Wg"))
                for b in range(NB):
                    rs = rstd_all.ap[:, b:b + 1]
                    hTs = hT[:, :, b * 128:(b + 1) * 128]
                    bk_ = nextF()
                    proj(hTs, hTB[b], lambda k: fox_Wg.ap[:, k, 1, :], 256, fox_Wg.b, bk_)
                    aug = qk_norm_aug(bk_, rs, b, G, kgain, False)
                    pb_ = nextB()
                    for hh in range(4):
                        tr(pb_.ap[0:68, hh * 128:(hh + 1) * 128], aug.ap[:, hh, :], [aug], [pb_], sig=(hh == 3))
                    cp(fox_KT.ap[0:68, :, b * 128:(b + 1) * 128], pb_.ap[0:68, 0:512].rearrange("p (h t) -> p h t", h=4), [pb_], [KTb[b]])
                    bv = nextF()
                    proj(hTs, hTB[b], lambda k: fox_Wg.ap[:, k, 2, :], 256, fox_Wg.b, bv)
                    act(fox_V.ap[:, b, :, 0:64], bv.ap[:, 0:256].rearrange("p (h d) -> p h d", h=4), AF.Copy, [bv, rstd_all], [Vb[b]], scale=rs)
                for b in range(NB):
                    rs = rstd_all.ap[:, b:b + 1]
                    hTs = hT[:, :, b * 128:(b + 1) * 128]
                    bq = nextF()
                    proj(hTs, hTB[b], lambda k: fox_Wg.ap[:, k, 0, :], 256, fox_Wg.b, bq)
                    aug = qk_norm_aug(bq, rs, b, G, qgain, True)
                    pb_ = nextB()
                    for hh in range(4):
                        tr(pb_.ap[0:68, hh * 128:(hh + 1) * 128], aug.ap[:, hh, :], [aug], [pb_], sig=(hh == 3))
                    cp(qT.ap[:], pb_.ap[0:68, 0:512].rearrange("p (h t) -> p h t", h=4), [pb_], [qT])
                    bg = nextF()
                    proj(hTs, hTB[b], lambda k: fox_Wg.ap[:, k, 3, :], 256, fox_Wg.b, bg)
                    ts(stats.ap[:, 2:3], rs, -1.0, None, ALU.mult, None, [rstd_all], [stats])
                    act(e_g.ap[:, 0:256], bg.ap[:, 0:256], AF.Exp, [bg, stats], [e_g], scale=stats.ap[:, 2:3])
                    ts(e_g.ap[:, 0:256], e_g.ap[:, 0:256], 1.0, None, ALU.add, None, [e_g], [e_g])
                    dve(lambda e: e.reciprocal(out=e_g.ap[:, 0:256], in_=e_g.ap[:, 0:256]), [e_g], [e_g])
                    stt(sg.ap[:, 0:256], bg.ap[:, 0:256], rs, e_g.ap[:, 0:256], ALU.mult, ALU.mult, [bg, rstd_all, e_g], [sg])
                    oacc = psF[5]
                    nkb = b + 1
                    for hh in range(4):
                        for j0 in range(0, nkb, 4):
                            js = list(range(j0, min(j0 + 4, nkb)))
                            bs = nextF()
                            for ji, jb in enumerate(js):
                                mm(bs.ap[:, ji * 128:(ji + 1) * 128], fox_KT.ap[0:68, hh, jb * 128:(jb + 1) * 128], qT.ap[:, hh, :],
                                   True, True, [KTb[jb], qT], [bs], sig=(ji == len(js) - 1))
                            pt = rot("PT", PT)
                            nj = len(js)
                            act(pt.ap[:, 0:nj, :], bs.ap[:, 0:nj * 128].rearrange("p (j t) -> p j t", j=nj), AF.Exp, [bs], [pt])
                            if js[-1] == b:
                                tt(pt.ap[:, nj - 1, :], pt.ap[:, nj - 1, :], maskT.ap[:], ALU.mult, [pt, maskT], [pt])
                            for ji, jb in enumerate(js):
                                last = (jb == b)
                                mm(oacc.ap[:, hh * 65:(hh + 1) * 65], pt.ap[:, ji, :], fox_V.ap[:, jb, hh, :], jb == 0, last,
                                   [pt, Vb[jb]], [oacc], sig=(last or ji == nj - 1))
                    o3 = oacc.ap[:, 0:260].rearrange("p (h c) -> p h c", h=4)
                    dve(lambda e: e.reciprocal(out=rden.ap[:].unsqueeze(2), in_=o3[:, :, 64:65]), [oacc], [rden])
                    tt(otmp.ap[:].rearrange("p (h d) -> p h d", h=4), o3[:, :, 0:64],
                       rden.ap[:].unsqueeze(2).to_broadcast([128, 4, 64]), ALU.mult, [oacc, rden], [otmp])
                    tt(og.ap[:, 0:256], otmp.ap[:], sg.ap[:, 0:256], ALU.mult, [otmp, sg], [og])
                    out_proj(b, [2 * G, 2 * G + 1])

        def final_pass(s):
            load_gbc(gbc_m, final_norm[0:1, :])
            stg = [e_g, sg]
            for b in range(NB):
                act(sqj.ap[:], h[:, b, :], AF.Square, [hB[b]], [sqj, stats], accum=stats.ap[:, 0:1])
                act(stats.ap[:, 1:2], stats.ap[:, 0:1], AF.Ln, [stats], [stats], bias=EPS, scale=1.0 / D)
                act(stats.ap[:, 3:4], stats.ap[:, 1:2], AF.Exp, [stats], [stats], scale=-0.5)
                t_ = stg[b % 2]
                stt(t_.ap[:], h[:, b, :], stats.ap[:, 3:4], gbc_m.ap[:], ALU.mult, ALU.mult, [hB[b], stats, gbc_m], [t_])
                tk.op("sp", lambda e, t_=t_, b=b: e.dma_start(out=out[s, b * 128:(b + 1) * 128, :], in_=t_.ap[:]),
                      [t_.b], [], dma=dmah(f"st{b % 2}"))

        def raw_store(s):
            for b in range(NB):
                tk.op("sp", lambda e, b=b: e.dma_start(out=out[s, b * 128:(b + 1) * 128, :], in_=h[:, b, :]),
                      [hB[b]], [], dma=dmah(f"st{b % 2}"))

        for s in range(nseq):
            for b in range(NB):
                tk.op("sp", lambda e, b=b, s=s: e.dma_start(out=h[:, b, :], in_=x[s, b * 128:(b + 1) * 128, :]), [], [hB[b]], dma=dmah("xload"))
            last = hB[NB - 1].w
            for b in range(NB):
                hB[b].w = last
            for i in layers:
                if i % 2 == 0:
                    gla_layer(i, s)
                else:
                    fox_layer(i, s)
                ple_pass(i, s)
            if final:
                final_pass(s)
            else:
                raw_store(s)
        for name in ("st0", "st1"):
            sem, stt_ = dmah(name)
            tk.prog["sp"].append(lambda e, sem=sem, v=stt_["val"]: e.wait_ge(sem, v))

        block = st.enter_context(nc.Block())

        @block.tensor
        def _(e):
            for f in tk.prog["pe"]:
                f(e)

        @block.scalar
        def _(e):
            for f in tk.prog["act"]:
                f(e)

        @block.vector
        def _(e):
            for f in tk.prog["dve"]:
                f(e)

        @block.gpsimd
        def _(e):
            for f in tk.prog["pool"]:
                f(e)

        @block.sync
        def _(e):
            for f in tk.prog["sp"]:
                f(e)
        build.stats = dict(tk.nins)
    return nc


_W = ("norm_mixer", "gla_w_in", "gla_w_a2", "gla_b_a", "gla_o_norm", "gla_w_out", "fox_w_in", "fox_b_f",
      "fox_q_norm", "fox_k_norm", "fox_w_out", "ple_proj", "ple_gate", "ple_norm")


def kernel(**inputs):
    n = 8
    x = np.ascontiguousarray(inputs["x"], dtype=np.float32)
    p = np.ascontiguousarray(inputs["p"], dtype=np.float32)
    per = x.shape[0] // n
    nc = build(nseq=per)
    shared = {k: np.ascontiguousarray(inputs[k], dtype=np.float32) for k in _W}
    shared["final_norm"] = np.ascontiguousarray(inputs["final_norm"], dtype=np.float32).reshape(1, D)
    in_maps = []
    for c in range(n):
        m = dict(shared)
        m["x"] = np.ascontiguousarray(x[c * per:(c + 1) * per])
        m["p"] = np.ascontiguousarray(p[:, c * per:(c + 1) * per])
        in_maps.append(m)
    res = run_bass_kernel_spmd(nc, in_maps, core_ids=list(range(n)))
    return np.concatenate([r["out"] for r in res.results], axis=0)
```

```python
import numpy as np
from contextlib import ExitStack
import concourse.bass as bass
import concourse.mybir as mybir
from concourse.bass_utils import run_bass_kernel_spmd

F32 = mybir.dt.float32
BF16 = mybir.dt.bfloat16
AF = mybir.ActivationFunctionType
ALU = mybir.AluOpType
AX = mybir.AxisListType

D = 1024
SEQ = 2048
NB = SEQ // 128
EPS = 1e-6


class Tok:
    __slots__ = ("sem", "val", "eng", "dma", "seq")


class Buf:
    def __init__(self, name):
        self.name = name
        self.w = None
        self.r = {}


class TK:
    EPOCH = 12000

    def __init__(self, nc, st):
        self.nc, self.st = nc, st
        self.prog = {e: [] for e in ("pe", "act", "dve", "pool", "sp")}
        self.sems, self.cnt, self.waited = {}, {}, {}
        self.pending = {e: [] for e in self.prog}
        self.nsem = 0
        self.seq = 0
        self.nins = {e: 0 for e in self.prog}
        for e in ("pe", "act", "dve"):
            self._new_epoch(e)

    def new_sem(self, name):
        self.nsem += 1
        return self.st.enter_context(self.nc.semaphore(f"{name}_{self.nsem}"))

    def new_dma(self, name):
        return (self.new_sem(name), {"val": 0})

    def _new_epoch(self, e):
        self.sems[e] = self.new_sem("s_" + e)
        self.cnt[e] = 0

    def op(self, eng, fn, reads=(), writes=(), sig=True, dma=None):
        deps = []
        for b in reads:
            if b.w is not None:
                deps.append(b.w)
        for b in writes:
            if b.w is not None:
                deps.append(b.w)
            for t in b.r.values():
                if (not t.dma) and t.eng == eng:
                    continue
                deps.append(t)
        prog = self.prog[eng]
        for d in deps:
            if (not d.dma) and d.eng == eng and eng == "pe":
                continue
            if d.val is None:
                raise RuntimeError("unresolved dependency token")
            key = (eng, id(d.sem))
            if self.waited.get(key, -1) >= d.val:
                continue
            self.waited[key] = d.val
            prog.append(lambda e, s=d.sem, v=d.val: e.wait_ge(s, v))
        tok = Tok()
        tok.eng = eng
        tok.dma = dma is not None
        self.seq += 1
        tok.seq = self.seq
        self.nins[eng] += 1
        if dma is not None:
            sem, stt = dma
            stt["val"] += 16
            tok.sem, tok.val = sem, stt["val"]
            prog.append(lambda e, f=fn, s=sem: f(e).then_inc(s, 16))
        elif sig:
            if self.cnt[eng] >= self.EPOCH:
                self._new_epoch(eng)
            self.cnt[eng] += 1
            tok.sem, tok.val = self.sems[eng], self.cnt[eng]
            for t in self.pending[eng]:
                t.sem, t.val = tok.sem, tok.val
            self.pending[eng] = []
            prog.append(lambda e, f=fn, s=tok.sem: f(e).then_inc(s, 1))
        else:
            tok.sem, tok.val = None, None
            self.pending[eng].append(tok)
            prog.append(lambda e, f=fn: f(e))
        for b in reads:
            b.r[id(tok.sem) if tok.dma else eng] = tok
        for b in writes:
            b.w = tok
            b.r = {}
        return tok

    def alias(self, old, new):
        merged = {}
        for ob in old:
            for t in ([ob.w] if ob.w is not None else []) + list(ob.r.values()):
                k = id(t.sem) if t.dma else t.eng
                if k not in merged or merged[k].seq < t.seq:
                    merged[k] = t
        for nb in new:
            nb.w = None
            nb.r = dict(merged)


class T:
    def __init__(self, ap, name):
        self.ap = ap
        self.b = Buf(name)


def build(nseq=2, layers=(0, 1, 2, 3), final=True):
    nc = bass.Bass("TRN2", target_bir_lowering=False)
    dt_in = lambda name, shape: nc.dram_tensor(name, list(shape), F32, kind="ExternalInput").ap()
    x = dt_in("x", [nseq, SEQ, D])
    p = dt_in("p", [4, nseq, SEQ, 256])
    norm_mixer = dt_in("norm_mixer", [4, D])
    gla_w_in = dt_in("gla_w_in", [2, D, 3088])
    gla_w_a2 = dt_in("gla_w_a2", [2, 16, 512])
    gla_b_a = dt_in("gla_b_a", [2, 512])
    gla_o_norm = dt_in("gla_o_norm", [2, 256])
    gla_w_out = dt_in("gla_w_out", [2, D, D])
    fox_w_in = dt_in("fox_w_in", [2, D, 4112])
    fox_b_f = dt_in("fox_b_f", [2, 16])
    fox_q_norm = dt_in("fox_q_norm", [2, 64])
    fox_k_norm = dt_in("fox_k_norm", [2, 64])
    fox_w_out = dt_in("fox_w_out", [2, D, D])
    ple_proj = dt_in("ple_proj", [4, 256, D])
    ple_gate = dt_in("ple_gate", [4, D, D])
    ple_norm = dt_in("ple_norm", [4, D])
    final_norm = dt_in("final_norm", [1, D])
    out = nc.dram_tensor("out", [nseq, SEQ, D], F32, kind="ExternalOutput").ap()

    with ExitStack() as st:
        tk = TK(nc, st)

        def sb(name, shape, dt):
            return st.enter_context(nc.sbuf_tensor(name, list(shape), dt))

        def sbt(name, shape, dt):
            return T(sb(name, shape, dt), name)

        h = sb("h", [128, NB, D], F32)
        hB = [Buf(f"h{b}") for b in range(NB)]
        arena = sb("arena", [128, 33792], BF16)
        arena2 = sb("arena2", [128, 16384], BF16)
        gla_Win = T(arena[:, 0:8 * 3088].rearrange("p (k c) -> p k c", k=8), "gla_Win")
        Wout = T(arena[:, 25600:25600 + 8192].rearrange("p (k c) -> p k c", k=8), "Wout")
        fox_Wg = T(arena[:, 0:8192].rearrange("p (k j c) -> p k j c", k=8, j=4), "fox_Wg")
        fox_KT = T(arena[:, 8192:16384].rearrange("p (h t) -> p h t", h=4), "fox_KT")
        fox_V = T(arena[:, 16384:16384 + 16 * 4 * 65].rearrange("p (b h c) -> p b h c", b=16, h=4), "fox_V")
        fox_Wf = T(arena[:, 20736:20736 + 128].rearrange("p (k c) -> p k c", k=8), "fox_Wf")
        ple_Wg = T(arena[:, 0:8192].rearrange("p (k c) -> p k c", k=8), "ple_Wg")
        ple_Wp = T(arena[:, 8192:10240].rearrange("p (k c) -> p k c", k=2), "ple_Wp")
        KTb = [Buf(f"KT{b}") for b in range(NB)]
        Vb = [Buf(f"V{b}") for b in range(NB)]
        arena_gla = [gla_Win.b]
        arena_fox = [fox_Wg.b, fox_Wf.b] + KTb + Vb
        arena_ple = [ple_Wg.b, ple_Wp.b]
        hT = arena2[:, :].rearrange("p (k t) -> p k t", k=8)
        hTB = [Buf(f"hT{b}") for b in range(NB)]
        a2 = [0]

        def carve(name, n_bf16, dt=BF16):
            ap_ = arena2[:, a2[0]:a2[0] + n_bf16]
            a2[0] += n_bf16
            if dt == F32:
                ap_ = ap_.bitcast(F32)
            return T(ap_, name)

        xe = carve("xe", 1024, F32)
        Lbf = carve("Lbf", 512)
        E_tm = carve("E_tm", 1024, F32)
        Einv_tm = carve("Einv_tm", 1024, F32)
        q_in = carve("q_in", 512)
        k_in = carve("k_in", 512)
        q_inT = carve("q_inT", 512)
        k_inT = carve("k_inT", 512)
        v_sb = carve("v_sb", 1024)
        attnT = carve("attnT", 512)
        S_f32 = carve("S_f32", 2048, F32)
        S_bf = carve("S_bf", 1024)
        gla_work = [xe, Lbf, E_tm, Einv_tm, q_in, k_in, q_inT, k_inT, v_sb, attnT, S_f32, S_bf]
        assert a2[0] <= 16384

        gbc_m = sbt("gbc_m", [128, D], F32)
        gbc_p = sbt("gbc_p", [128, D], F32)
        hb = [sbt(f"hb{i}", [128, D], BF16) for i in range(2)]
        sqj = sbt("sqj", [128, D], BF16)
        hTb = [sbt(f"hTb{i}", [128, 8, 128], BF16) for i in range(2)]
        e_g = sbt("e_g", [128, D], F32)
        sg = sbt("sg", [128, D], F32)
        sgF = []
        for i_ in range(2):
            t_ = T(sg.ap[:, i_ * 256:(i_ + 1) * 256], f"sgF{i_}")
            t_.b = sg.b
            sgF.append(t_)
        og = sbt("og", [128, D], BF16)
        ogT = sbt("ogT", [128, 8, 128], BF16)
        p_b = [sbt(f"p_b{i}", [128, 256], BF16) for i in range(2)]
        pT2 = [sbt(f"pT{i}", [128, 2, 128], BF16) for i in range(2)]
        stats = sbt("stats", [128, 64], F32)
        rstd_all = sbt("rstd_all", [128, NB], F32)
        ident = sbt("ident", [128, 128], BF16)
        maskT = sbt("maskT", [128, 128], BF16)
        tri32 = sbt("tri32", [128, 128], F32)
        ones32 = sbt("ones32", [128, 128], F32)
        ones_bf = sbt("ones_bf", [128, 1], BF16)
        a_sb = sbt("a_sb", [128, 16], BF16)
        aT = sbt("aT", [17, 128], BF16)
        wa2 = sbt("wa2", [17, 512], BF16)
        gcol = sbt("gcol", [128, 2], F32)
        decay = sbt("decay", [128, 4], F32)
        sso = sbt("sso", [128, 8], F32)
        kq_aug = [sbt(f"kq_aug{i}", [128, 4, 68], BF16) for i in range(2)]
        qT2 = [sbt(f"qT{i}", [68, 4, 128], BF16) for i in range(2)]
        PT = [sbt(f"PT{i}", [128, 4, 128], BF16) for i in range(2)]
        sqk = sbt("sqk", [128, 256], F32)
        fst = sbt("fst", [128, 64], F32)
        kgain = sbt("kgain", [128, 64], F32)
        qgain = sbt("qgain", [128, 64], F32)
        bf_bc = sbt("bf_bc", [128, 16], F32)
        Lf = sbt("Lf", [128, 16], F32)
        Rf = sbt("Rf", [128, 16], F32)
        C_all = sbt("C_all", [128, NB, 16], F32)
        Chl = sbt("Chl", [128, NB, 2, 16], BF16)
        Ctmp = sbt("Ctmp", [128, 16], F32)
        CB = [Buf(f"C{b}") for b in range(NB)]
        rden = sbt("rden", [128, 4], F32)
        otmp = sbt("otmp", [128, 256], F32)

        psF = [T(st.enter_context(nc.psum_tensor(f"psF{i}", [128, 512], F32)), f"psF{i}") for i in range(6)]
        psB = [T(st.enter_context(nc.psum_tensor(f"psB{i}", [128, 1024], BF16)), f"psB{i}") for i in range(2)]
        rr = {"F": 0, "B": 0, "hb": 0, "hTb": 0, "pb": 0, "PT": 0, "aug": 0}

        def nextF(n=5):
            rr["F"] = (rr["F"] + 1) % n
            return psF[rr["F"]]

        def nextB():
            rr["B"] ^= 1
            return psB[rr["B"]]

        def rot(key, lst):
            rr[key] = (rr[key] + 1) % len(lst)
            return lst[rr[key]]

        def bl(xs):
            return [t.b if isinstance(t, T) else t for t in xs]

        def mm(o, lhsT, rhs, start, stop, reads, writes, sig=None):
            tk.op("pe", lambda e: e.matmul(o, lhsT=lhsT, rhs=rhs, start=start, stop=stop),
                  bl(reads), bl(writes), sig=(stop if sig is None else sig))

        def tr(o, i, reads, writes, sig=True):
            tk.op("pe", lambda e: e.transpose(out=o, in_=i, identity=ident.ap[:]), bl(reads) + [ident.b], bl(writes), sig=sig)

        def act(o, i, func, reads, writes, bias=None, scale=None, accum=None):
            kw = {}
            if bias is not None:
                kw["bias"] = bias
            if scale is not None:
                kw["scale"] = scale
            if accum is not None:
                kw["accum_out"] = accum
            tk.op("act", lambda e: e.activation(out=o, in_=i, func=func, **kw), bl(reads), bl(writes))

        def dve(fn, reads, writes):
            tk.op("dve", fn, bl(reads), bl(writes))

        def tt(o, a, b_, op, reads, writes):
            dve(lambda e: e.tensor_tensor(out=o, in0=a, in1=b_, op=op), reads, writes)

        def ts(o, a, s1, s2, op0, op1, reads, writes):
            if s2 is None:
                dve(lambda e: e.tensor_scalar(out=o, in0=a, scalar1=s1, scalar2=None, op0=op0), reads, writes)
            else:
                dve(lambda e: e.tensor_scalar(out=o, in0=a, scalar1=s1, scalar2=s2, op0=op0, op1=op1), reads, writes)

        def stt(o, a, s, b_, op0, op1, reads, writes):
            dve(lambda e: e.scalar_tensor_tensor(out=o, in0=a, scalar=s, in1=b_, op0=op0, op1=op1), reads, writes)

        def cp(o, i, reads, writes):
            dve(lambda e: e.tensor_copy(out=o, in_=i), reads, writes)

        def wload(o, i, writes, dmah):
            tk.op("pool", lambda e: e.dma_start(out=o, in_=i), [], bl(writes), dma=dmah)

        def wload_nc(o, i, writes, dmah):
            tk.op("pool", lambda e: e.dma_start(out=o, in_=i, allow_slow_non_contiguous=True), [], bl(writes), dma=dmah)

        dm = {}

        def dmah(name):
            if name not in dm:
                dm[name] = tk.new_dma(name)
            return dm[name]

        pl = lambda fn, reads, writes: tk.op("dve", fn, bl(reads), bl(writes))
        tk._new_epoch("pool")

        def pool(fn, reads, writes):
            tk.op("pool", fn, bl(reads), bl(writes))

        pool(lambda e: e.memset(ones32.ap[:], 1.0), [], [ones32])
        pool(lambda e: e.memset(tri32.ap[:], 1.0), [], [tri32])
        pool(lambda e: e.affine_select(out=tri32.ap[:], in_=tri32.ap[:], pattern=[[1, 128]], compare_op=ALU.is_ge,
                                       fill=0.0, base=0, channel_multiplier=-1), [tri32], [tri32])
        cfv = e_g.ap[:, 0:128]
        pool(lambda e: e.memset(cfv, 1.0), [], [e_g])
        pool(lambda e: e.affine_select(out=cfv, in_=cfv, pattern=[[-1, 128]], compare_op=ALU.is_equal,
                                       fill=0.0, base=0, channel_multiplier=1), [e_g], [e_g])
        cp(ident.ap[:], cfv, [e_g], [ident])
        cp(maskT.ap[:], tri32.ap[:], [tri32], [maskT])
        cp(ones_bf.ap[:], ones32.ap[:, 0:1], [ones32], [ones_bf])
        dve(lambda e: e.memset(aT.ap[:], 1.0), [], [aT])

        def pass0(b, gbc, rstd_ap, hT_dst, hT_buf):
            hbt = rot("hb", hb)
            act(sqj.ap[:], h[:, b, :], AF.Square, [hB[b]], [sqj, stats], accum=stats.ap[:, 0:1])
            act(stats.ap[:, 1:2], stats.ap[:, 0:1], AF.Ln, [stats], [stats], bias=EPS, scale=1.0 / D)
            act(rstd_ap, stats.ap[:, 1:2], AF.Exp, [stats], [rstd_all], scale=-0.5)
            tt(hbt.ap[:], h[:, b, :], gbc.ap[:], ALU.mult, [hB[b], gbc], [hbt])
            pb_ = nextB()
            for k in range(8):
                tr(pb_.ap[:, k * 128:(k + 1) * 128], hbt.ap[:, k * 128:(k + 1) * 128], [hbt], [pb_], sig=(k == 7))
            cp(hT_dst, pb_.ap[:, :].rearrange("p (k t) -> p k t", k=8), [pb_], [hT_buf])

        def load_gbc(dst, src_row):
            wload(dst.ap[:], src_row.to_broadcast([128, D]), [dst], dmah(dst.b.name))

        def proj(hT_ap, hT_buf, W_ap_fn, ncols, Wbuf, bank):
            for k in range(8):
                mm(bank.ap[:, 0:ncols], hT_ap[:, k, :], W_ap_fn(k), k == 0, k == 7, [hT_buf, Wbuf], [bank])

        def sigmoid_act(dst, src, nrs_ap, reads, wbuf):
            act(dst, src, AF.Exp, reads, [wbuf], scale=nrs_ap)
            act(dst, dst, AF.Ln, [wbuf], [wbuf], bias=1.0)
            act(dst, dst, AF.Exp, [wbuf], [wbuf], scale=-1.0)

        def ple_pass(i, s):
            tk.alias(arena_gla + arena_fox, arena_ple)
            wg_view = ple_gate[i].rearrange("(k p) c -> p k c", p=128)
            for k in range(8):
                wload(ple_Wg.ap[:, k, :], wg_view[:, k, :], [ple_Wg], dmah("ple_Wg"))
            wload(ple_Wp.ap[:, :, :], ple_proj[i].rearrange("(k p) c -> p k c", p=128), [ple_Wp], dmah("ple_Wp"))
            load_gbc(gbc_p, ple_norm[i:i + 1, :])

            def stA(b):
                rs = rstd_all.ap[:, b:b + 1]
                hTt = hTb[b % 2]
                pbt = p_b[b % 2]
                pTt = pT2[b % 2]
                wload(pbt.ap[:], p[i, s, b * 128:(b + 1) * 128, :], [pbt], dmah(pbt.b.name))
                pass0(b, gbc_p, rs, hTt.ap[:], hTt.b)
                pb_ = nextB()
                for k in range(2):
                    tr(pb_.ap[:, k * 128:(k + 1) * 128], pbt.ap[:, k * 128:(k + 1) * 128], [pbt], [pb_], sig=(k == 1))
                cp(pTt.ap[:], pb_.ap[:, 0:256].rearrange("p (k t) -> p k t", k=2), [pb_], [pTt])

            def stB(b):
                rs = rstd_all.ap[:, b:b + 1]
                hTt = hTb[b % 2]
                pTt = pT2[b % 2]
                ts(stats.ap[:, 2:3], rs, -1.0, None, ALU.mult, None, [rstd_all], [stats])
                for cg in range(2):
                    cs = slice(cg * 512, (cg + 1) * 512)
                    bg = nextF()
                    proj(hTt.ap, hTt.b, lambda k: ple_Wg.ap[:, k, cs], 512, ple_Wg.b, bg)
                    sigmoid_act(sg.ap[:, cs], bg.ap[:], stats.ap[:, 2:3], [bg, stats], sg)
                    bp = nextF()
                    for k in range(2):
                        mm(bp.ap[:], pTt.ap[:, k, :], ple_Wp.ap[:, k, cs], k == 0, k == 1, [pTt, ple_Wp], [bp])
                    tt(e_g.ap[:, cs], bp.ap[:], sg.ap[:, cs], ALU.mult, [bp, sg], [e_g])
                    tt(h[:, b, cs], h[:, b, cs], e_g.ap[:, cs], ALU.add, [hB[b], e_g], [hB[b]])

            stA(0)
            for b in range(NB):
                if b + 1 < NB:
                    stA(b + 1)
                stB(b)

        def gla_layer(i, s):
            j = i // 2
            tk.alias(arena_ple + arena_fox, arena_gla)
            tk.alias(hTB, [t.b for t in gla_work])
            wv = gla_w_in[j].rearrange("(k p) c -> p k c", p=128)
            for (c0, c1) in ((3072, 3088), (0, 512), (512, 1024), (1024, 1536), (1536, 2048), (2048, 2560), (2560, 3072)):
                for k0 in (0, 4):
                    wload(gla_Win.ap[:, k0:k0 + 4, c0:c1], wv[:, k0:k0 + 4, c0:c1], [gla_Win], dmah("gla_Win"))
            wo = gla_w_out[j].rearrange("(k p) c -> p k c", p=128)
            for k0 in (0, 4):
                wload(Wout.ap[:, k0:k0 + 4, :], wo[:, k0:k0 + 4, :], [Wout], dmah("Wout"))
            wload(wa2.ap[0:16, :], gla_w_a2[j], [wa2], dmah("wa2"))
            wload(wa2.ap[16:17, :], gla_b_a[j:j + 1, :], [wa2], dmah("wa2"))
            tk.op("sp", lambda e: e.dma_start(out=gcol.ap[:], in_=gla_o_norm[j].rearrange("(a p) -> p a", p=128),
                                              allow_slow_non_contiguous=True), [], [gcol.b], dma=dmah("gcol"))
            load_gbc(gbc_m, norm_mixer[i:i + 1, :])
            for k in range(8):
                ts(Wout.ap[:, k, :], Wout.ap[:, k, :], gcol.ap[:, (k % 2):(k % 2) + 1], None, ALU.mult, None, [Wout, gcol], [Wout])
            Win = gla_Win
            for b in range(NB):
                rs = rstd_all.ap[:, b:b + 1]
                hTt = rot("hTb", hTb)
                pass0(b, gbc_m, rs, hTt.ap[:], hTt.b)
                ba = nextF()
                proj(hTt.ap, hTt.b, lambda k: Win.ap[:, k, 3072:3088], 16, Win.b, ba)
                act(a_sb.ap[:], ba.ap[:, 0:16], AF.Copy, [ba, rstd_all], [a_sb], scale=rs)
                pb_ = nextB()
                tr(pb_.ap[0:16, 0:128], a_sb.ap[:], [a_sb], [pb_])
                cp(aT.ap[0:16, :], pb_.ap[0:16, 0:128], [pb_], [aT])
                bx = nextF()
                mm(bx.ap[:], aT.ap[:], wa2.ap[:], True, True, [aT, wa2], [bx])
                act(xe.ap[:], bx.ap[:], AF.Exp, [bx], [xe], scale=-1.0)
                act(xe.ap[:], xe.ap[:], AF.Ln, [xe], [xe], bias=1.0)
                cp(Lbf.ap[:], xe.ap[:], [xe], [Lbf])
                bc = nextF()
                mm(bc.ap[:], maskT.ap[:], Lbf.ap[:], True, True, [maskT, Lbf], [bc])
                act(E_tm.ap[:], bc.ap[:], AF.Exp, [bc], [E_tm], scale=-1.0 / 16, bias=float(np.log(128.0 ** -0.5)))
                act(Einv_tm.ap[:], bc.ap[:], AF.Exp, [bc], [Einv_tm], scale=1.0 / 16)
                bd = nextF()
                for hh in range(4):
                    mm(bd.ap[:, hh:hh + 1], Lbf.ap[:, hh * 128:(hh + 1) * 128], ones_bf.ap[:], True, True, [Lbf, ones_bf], [bd], sig=(hh == 3))
                act(decay.ap[:], bd.ap[:, 0:4], AF.Exp, [bd], [decay], scale=-1.0 / 16)
                bq = nextF()
                proj(hTt.ap, hTt.b, lambda k: Win.ap[:, k, 0:512], 512, Win.b, bq)
                stt(q_in.ap[:], bq.ap[:], rs, E_tm.ap[:], ALU.mult, ALU.mult, [bq, rstd_all, E_tm], [q_in])
                bk_ = nextF()
                proj(hTt.ap, hTt.b, lambda k: Win.ap[:, k, 512:1024], 512, Win.b, bk_)
                stt(k_in.ap[:], bk_.ap[:], rs, Einv_tm.ap[:], ALU.mult, ALU.mult, [bk_, rstd_all, Einv_tm], [k_in])
                for (src, dst) in ((q_in, q_inT), (k_in, k_inT)):
                    pb_ = nextB()
                    for hh in range(4):
                        tr(pb_.ap[:, hh * 128:(hh + 1) * 128], src.ap[:, hh * 128:(hh + 1) * 128], [src], [pb_], sig=(hh == 3))
                    cp(dst.ap[:], pb_.ap[:, 0:512], [pb_], [dst])
                for cg in range(2):
                    bv = nextF()
                    proj(hTt.ap, hTt.b, lambda k: Win.ap[:, k, 1024 + cg * 512:1536 + cg * 512], 512, Win.b, bv)
                    act(v_sb.ap[:, cg * 512:(cg + 1) * 512], bv.ap[:], AF.Copy, [bv, rstd_all], [v_sb], scale=rs)
                bt = nextF()
                for hh in range(4):
                    mm(bt.ap[:, hh * 128:(hh + 1) * 128], k_inT.ap[:, hh * 128:(hh + 1) * 128], q_inT.ap[:, hh * 128:(hh + 1) * 128],
                       True, True, [k_inT, q_inT], [bt], sig=(hh == 3))
                tt(attnT.ap[:].rearrange("p (h c) -> p h c", h=4), bt.ap[:].rearrange("p (h c) -> p h c", h=4),
                   maskT.ap[:].unsqueeze(1).to_broadcast([128, 4, 128]), ALU.mult, [bt, maskT], [attnT])
                bo = [nextF(), nextF()]
                for hh in range(4):
                    o_ap = bo[hh // 2].ap[:, (hh % 2) * 256:(hh % 2) * 256 + 256]
                    mm(o_ap, attnT.ap[:, hh * 128:(hh + 1) * 128], v_sb.ap[:, hh * 256:(hh + 1) * 256], True, b == 0,
                       [attnT, v_sb], [bo[hh // 2]], sig=(b == 0 and hh % 2 == 1))
                    if b > 0:
                        mm(o_ap, q_inT.ap[:, hh * 128:(hh + 1) * 128], S_bf.ap[:, hh * 256:(hh + 1) * 256], False, True,
                           [q_inT, S_bf], [bo[hh // 2]], sig=(hh % 2 == 1))
                for hh in range(4):
                    o_ap = bo[hh // 2].ap[:, (hh % 2) * 256:(hh % 2) * 256 + 256]
                    act(sqj.ap[:, 0:256], o_ap, AF.Square, [bo[hh // 2]], [sqj, sso], accum=sso.ap[:, hh:hh + 1])
                act(sso.ap[:, 4:8], sso.ap[:, 0:4], AF.Ln, [sso], [sso], bias=EPS, scale=1.0 / 256)
                act(sso.ap[:, 0:4], sso.ap[:, 4:8], AF.Exp, [sso], [sso], scale=-0.5)
                ts(stats.ap[:, 2:3], rs, -1.0, None, ALU.mult, None, [rstd_all], [stats])
                for cg in range(2):
                    cs = slice(cg * 512, (cg + 1) * 512)
                    bg = nextF()
                    proj(hTt.ap, hTt.b, lambda k: Win.ap[:, k, 2048 + cg * 512:2560 + cg * 512], 512, Win.b, bg)
                    act(e_g.ap[:, cs], bg.ap[:], AF.Exp, [bg, stats], [e_g], scale=stats.ap[:, 2:3])
                    ts(e_g.ap[:, cs], e_g.ap[:, cs], 1.0, None, ALU.add, None, [e_g], [e_g])
                    dve(lambda e, cs=cs: e.reciprocal(out=e_g.ap[:, cs], in_=e_g.ap[:, cs]), [e_g], [e_g])
                    stt(sg.ap[:, cs], bg.ap[:], rs, e_g.ap[:, cs], ALU.mult, ALU.mult, [bg, rstd_all, e_g], [sg])
                for hh in range(4):
                    o_ap = bo[hh // 2].ap[:, (hh % 2) * 256:(hh % 2) * 256 + 256]
                    stt(og.ap[:, hh * 256:(hh + 1) * 256], o_ap, sso.ap[:, hh:hh + 1], sg.ap[:, hh * 256:(hh + 1) * 256],
                        ALU.mult, ALU.mult, [bo[hh // 2], sso, sg], [og])
                if b < NB - 1:
                    for half in range(2):
                        bs = nextF()
                        for hq in range(2):
                            hh = half * 2 + hq
                            mm(bs.ap[:, hq * 256:(hq + 1) * 256], k_in.ap[:, hh * 128:(hh + 1) * 128], v_sb.ap[:, hh * 256:(hh + 1) * 256],
                               True, True, [k_in, v_sb], [bs], sig=(hq == 1))
                        Sv = S_f32.ap[:, half * 512:(half + 1) * 512]
                        if b == 0:
                            cp(Sv, bs.ap[:], [bs], [S_f32])
                        else:
                            tt(Sv, Sv, bs.ap[:], ALU.add, [bs, S_f32], [S_f32])
                        for hq in range(2):
                            hh = half * 2 + hq
                            Sh = S_f32.ap[:, hh * 256:(hh + 1) * 256]
                            ts(Sh, Sh, decay.ap[:, hh:hh + 1], None, ALU.mult, None, [S_f32, decay], [S_f32])
                    act(S_bf.ap[:], S_f32.ap[:], AF.Copy, [S_f32], [S_bf])
                out_proj(b, range(8))

        def out_proj(b, chunks, nf=5):
            chunks = list(chunks)
            n = len(chunks)
            pb_ = nextB()
            for ci, k in enumerate(chunks):
                tr(pb_.ap[:, ci * 128:(ci + 1) * 128], og.ap[:, ci * 128:(ci + 1) * 128], [og], [pb_], sig=(ci == n - 1))
            dve(lambda e: e.tensor_copy(out=ogT.ap[:, 0:n, :], in_=pb_.ap[:, 0:n * 128].rearrange("p (k t) -> p k t", k=n)), [pb_], [ogT])
            for cg in range(2):
                cs = slice(cg * 512, (cg + 1) * 512)
                by = nextF(nf)
                for ci, k in enumerate(chunks):
                    mm(by.ap[:], ogT.ap[:, ci, :], Wout.ap[:, k, cs], ci == 0, ci == n - 1, [ogT, Wout], [by])
                tt(h[:, b, cs], h[:, b, cs], by.ap[:], ALU.add, [hB[b], by], [hB[b]])

        def fox_layer(i, s):
            j = i // 2
            tk.alias(arena_ple + arena_gla, arena_fox)
            tk.alias([t.b for t in gla_work], hTB)
            wv = fox_w_in[j].rearrange("(k p) c -> p k c", p=128)
            wo = fox_w_out[j].rearrange("(k p) c -> p k c", p=128)
            for k0 in (0, 4):
                wload(Wout.ap[:, k0:k0 + 4, :], wo[:, k0:k0 + 4, :], [Wout], dmah("Wout"))
            wload(fox_Wf.ap[:, :, :], wv[:, :, 4096:4112], [fox_Wf], dmah("fox_Wf"))
            load_gbc(gbc_m, norm_mixer[i:i + 1, :])
            wload(kgain.ap[:], fox_k_norm[j:j + 1, :].to_broadcast([128, 64]), [kgain], dmah("kgain"))
            wload(qgain.ap[:], fox_q_norm[j:j + 1, :].to_broadcast([128, 64]), [qgain], dmah("qgain"))
            wload(bf_bc.ap[:], fox_b_f[j:j + 1, :].to_broadcast([128, 16]), [bf_bc], dmah("bf_bc"))
            ts(qgain.ap[:], qgain.ap[:], 0.125, None, ALU.mult, None, [qgain], [qgain])
            COLB = (0, 3072, 1024, 2048)

            def load_group(G):
                for jj in range(4):
                    c0 = COLB[jj] + G * 256
                    wload(fox_Wg.ap[:, :, jj, :], wv[:, :, c0:c0 + 256], [fox_Wg], dmah("fox_Wg"))

            load_group(0)
            dve(lambda e: e.memset(Rf.ap[:], 0.0), [], [Rf])
            for b in range(NB):
                rs = rstd_all.ap[:, b:b + 1]
                pass0(b, gbc_m, rs, hT[:, :, b * 128:(b + 1) * 128], hTB[b])
                bf_ = nextF(4)
                proj(hT[:, :, b * 128:(b + 1) * 128], hTB[b], lambda k: fox_Wf.ap[:, k, :], 16, fox_Wf.b, bf_)
                stt(Lf.ap[:], bf_.ap[:, 0:16], rs, bf_bc.ap[:], ALU.mult, ALU.add, [bf_, rstd_all, bf_bc], [Lf])
                act(Lf.ap[:], Lf.ap[:], AF.Exp, [Lf], [Lf], scale=-1.0)
                act(Lf.ap[:], Lf.ap[:], AF.Ln, [Lf], [Lf], bias=1.0)
                bc = nextF(4)
                mm(bc.ap[:, 0:16], tri32.ap[:], Lf.ap[:], True, False, [tri32, Lf], [bc], sig=False)
                mm(bc.ap[:, 0:16], ones32.ap[:], Rf.ap[:], False, True, [ones32, Rf], [bc])
                cp(C_all.ap[:, b, :], bc.ap[:, 0:16], [bc], [CB[b]])
                tt(Rf.ap[:], Rf.ap[:], Lf.ap[:], ALU.add, [Rf, Lf], [Rf])
                cp(Chl.ap[:, b, 0, :], C_all.ap[:, b, :], [CB[b]], [CB[b]])
                tt(Ctmp.ap[:], C_all.ap[:, b, :], Chl.ap[:, b, 0, :], ALU.subtract, [CB[b]], [Ctmp])
                cp(Chl.ap[:, b, 1, :], Ctmp.ap[:], [Ctmp], [CB[b]])
            dve(lambda e: e.memset(fox_V.ap[:, :, :, 64:65], 1.0), [], Vb)

            def qk_norm_aug(bank, rs, b, G, gain, is_q, aug):
                act(sqk.ap[:], bank.ap[:, 0:256], AF.Square, [bank], [sqk])
                dve(lambda e: e.reduce_sum(out=fst.ap[:, 0:4], in_=sqk.ap[:].rearrange("p (h d) -> p h d", h=4), axis=AX.X), [sqk], [fst])
                tt(fst.ap[:, 8:9], rs, rs, ALU.mult, [rstd_all], [fst])
                ts(fst.ap[:, 4:8], fst.ap[:, 0:4], fst.ap[:, 8:9], None, ALU.mult, None, [fst], [fst])
                act(fst.ap[:, 12:16], fst.ap[:, 4:8], AF.Ln, [fst], [fst], bias=EPS, scale=1.0 / 64)
                act(fst.ap[:, 16:20], fst.ap[:, 12:16], AF.Exp, [fst], [fst], scale=-0.5)
                ts(fst.ap[:, 20:24], fst.ap[:, 16:20], rs, None, ALU.mult, None, [fst, rstd_all], [fst])
                tt(sqk.ap[:].rearrange("p (h d) -> p h d", h=4), bank.ap[:, 0:256].rearrange("p (h d) -> p h d", h=4),
                   fst.ap[:, 20:24].unsqueeze(2).to_broadcast([128, 4, 64]), ALU.mult, [bank, fst], [sqk])
                tt(aug.ap[:, :, 0:64], sqk.ap[:].rearrange("p (h d) -> p h d", h=4),
                   gain.ap[:].unsqueeze(1).to_broadcast([128, 4, 64]), ALU.mult, [sqk, gain], [aug])
                hs = slice(G * 4, G * 4 + 4)
                if is_q:
                    ts(aug.ap[:, :, 64:66], Chl.ap[:, b, :, hs].rearrange("p a h -> p h a"), -1.0, None, ALU.mult, None, [CB[b]], [aug])
                    dve(lambda e: e.memset(aug.ap[:, :, 66:68], 1.0), [], [aug])
                else:
                    dve(lambda e: e.memset(aug.ap[:, :, 64:66], 1.0), [], [aug])
                    cp(aug.ap[:, :, 66:68], Chl.ap[:, b, :, hs].rearrange("p a h -> p h a"), [CB[b]], [aug])

            for G in range(4):
                if G > 0:
                    load_group(G)

                def p1A(b):
                    rs = rstd_all.ap[:, b:b + 1]
                    hTs = hT[:, :, b * 128:(b + 1) * 128]
                    bk_ = nextF(4)
                    proj(hTs, hTB[b], lambda k: fox_Wg.ap[:, k, 2:4, :], 512, fox_Wg.b, bk_)
                    qk_norm_aug(bk_, rs, b, G, kgain, False, kq_aug[b % 2])
                    act(fox_V.ap[:, b, :, 0:64], bk_.ap[:, 256:512].rearrange("p (h d) -> p h d", h=4), AF.Copy, [bk_, rstd_all], [Vb[b]], scale=rs)

                def p1B(b):
                    aug = kq_aug[b % 2]
                    pb_ = nextB()
                    for hh in range(4):
                        tr(pb_.ap[0:68, hh * 128:(hh + 1) * 128], aug.ap[:, hh, :], [aug], [pb_], sig=(hh == 3))
                    cp(fox_KT.ap[0:68, :, b * 128:(b + 1) * 128], pb_.ap[0:68, 0:512].rearrange("p (h t) -> p h t", h=4), [pb_], [KTb[b]])

                p1A(0)
                for b in range(NB):
                    if b + 1 < NB:
                        p1A(b + 1)
                    p1B(b)

                def front(b):
                    rs = rstd_all.ap[:, b:b + 1]
                    hTs = hT[:, :, b * 128:(b + 1) * 128]
                    bq = nextF(4)
                    proj(hTs, hTB[b], lambda k: fox_Wg.ap[:, k, 0:2, :], 512, fox_Wg.b, bq)
                    qk_norm_aug(bq, rs, b, G, qgain, True, kq_aug[b % 2])
                    ts(stats.ap[:, 2:3], rs, -1.0, None, ALU.mult, None, [rstd_all], [stats])
                    sigmoid_act(e_g.ap[:, 0:256], bq.ap[:, 256:512], stats.ap[:, 2:3], [bq, stats], e_g)
                    sgt = sgF[b % 2]
                    stt(sgt.ap[:], bq.ap[:, 256:512], rs, e_g.ap[:, 0:256], ALU.mult, ALU.mult, [bq, rstd_all, e_g], [sgt])

                def frontB(b):
                    aug = kq_aug[b % 2]
                    qTt = qT2[b % 2]
                    pb_ = nextB()
                    for hh in range(4):
                        tr(pb_.ap[0:68, hh * 128:(hh + 1) * 128], aug.ap[:, hh, :], [aug], [pb_], sig=(hh == 3))
                    cp(qTt.ap[:], pb_.ap[0:68, 0:512].rearrange("p (h t) -> p h t", h=4), [pb_], [qTt])

                def attn(b):
                    qTt = qT2[b % 2]
                    oacc = psF[4 + (b % 2)]
                    banks = {}

                    def S(jb):
                        bs = nextF(4)
                        banks[jb] = bs
                        for hh in range(4):
                            mm(bs.ap[:, hh * 128:(hh + 1) * 128], fox_KT.ap[0:68, hh, jb * 128:(jb + 1) * 128], qTt.ap[:, hh, :],
                               True, True, [KTb[jb], qTt], [bs], sig=(hh == 3))

                    def P(jb):
                        bs = banks.pop(jb)
                        pt = rot("PT", PT)
                        act(pt.ap[:], bs.ap[:].rearrange("p (j t) -> p j t", j=4), AF.Exp, [bs], [pt])
                        if jb == b:
                            tt(pt.ap[:], pt.ap[:], maskT.ap[:].unsqueeze(1).to_broadcast([128, 4, 128]), ALU.mult, [pt, maskT], [pt])
                        for hh in range(4):
                            mm(oacc.ap[:, hh * 65:(hh + 1) * 65], pt.ap[:, hh, :], fox_V.ap[:, jb, hh, :], (jb == 0 and hh == 0), (jb == b and hh == 3),
                               [pt, Vb[jb]], [oacc], sig=(hh == 3))

                    S(0)
                    for jb in range(b + 1):
                        if jb + 1 <= b:
                            S(jb + 1)
                        P(jb)

                def backD(b):
                    oacc = psF[4 + (b % 2)]
                    o3 = oacc.ap[:, 0:260].rearrange("p (h c) -> p h c", h=4)
                    dve(lambda e: e.reciprocal(out=rden.ap[:].unsqueeze(2), in_=o3[:, :, 64:65]), [oacc], [rden])
                    tt(otmp.ap[:].rearrange("p (h d) -> p h d", h=4), o3[:, :, 0:64],
                       rden.ap[:].unsqueeze(2).to_broadcast([128, 4, 64]), ALU.mult, [oacc, rden], [otmp])
                    tt(og.ap[:, 0:256], otmp.ap[:], sgF[b % 2].ap[:], ALU.mult, [otmp, sgF[b % 2]], [og])

                def backP(b):
                    out_proj(b, [2 * G, 2 * G + 1], nf=4)

                front(0)
                frontB(0)
                for b in range(NB):
                    if b >= 1:
                        backD(b - 1)
                    if b + 1 < NB:
                        front(b + 1)
                    attn(b)
                    if b + 1 < NB:
                        frontB(b + 1)
                    if b >= 1:
                        backP(b - 1)
                backD(NB - 1)
                backP(NB - 1)

        def final_pass(s):
            load_gbc(gbc_m, final_norm[0:1, :])
            stg = [e_g, sg]
            for b in range(NB):
                act(sqj.ap[:], h[:, b, :], AF.Square, [hB[b]], [sqj, stats], accum=stats.ap[:, 0:1])
                act(stats.ap[:, 1:2], stats.ap[:, 0:1], AF.Ln, [stats], [stats], bias=EPS, scale=1.0 / D)
                act(stats.ap[:, 3:4], stats.ap[:, 1:2], AF.Exp, [stats], [stats], scale=-0.5)
                t_ = stg[b % 2]
                stt(t_.ap[:], h[:, b, :], stats.ap[:, 3:4], gbc_m.ap[:], ALU.mult, ALU.mult, [hB[b], stats, gbc_m], [t_])
                tk.op("sp", lambda e, t_=t_, b=b: e.dma_start(out=out[s, b * 128:(b + 1) * 128, :], in_=t_.ap[:]),
                      [t_.b], [], dma=dmah(f"st{b % 2}"))

        def raw_store(s):
            for b in range(NB):
                tk.op("sp", lambda e, b=b: e.dma_start(out=out[s, b * 128:(b + 1) * 128, :], in_=h[:, b, :]),
                      [hB[b]], [], dma=dmah(f"st{b % 2}"))

        for s in range(nseq):
            for b in range(NB):
                tk.op("sp", lambda e, b=b, s=s: e.dma_start(out=h[:, b, :], in_=x[s, b * 128:(b + 1) * 128, :]), [], [hB[b]], dma=dmah("xload"))
            last = hB[NB - 1].w
            for b in range(NB):
                hB[b].w = last
            for i in layers:
                if i % 2 == 0:
                    gla_layer(i, s)
                else:
                    fox_layer(i, s)
                ple_pass(i, s)
            if final:
                final_pass(s)
            else:
                raw_store(s)
        for name in ("st0", "st1"):
            sem, stt_ = dmah(name)
            tk.prog["sp"].append(lambda e, sem=sem, v=stt_["val"]: e.wait_ge(sem, v))

        block = st.enter_context(nc.Block())

        @block.tensor
        def _(e):
            for f in tk.prog["pe"]:
                f(e)

        @block.scalar
        def _(e):
            for f in tk.prog["act"]:
                f(e)

        @block.vector
        def _(e):
            for f in tk.prog["dve"]:
                f(e)

        @block.gpsimd
        def _(e):
            for f in tk.prog["pool"]:
                f(e)

        @block.sync
        def _(e):
            for f in tk.prog["sp"]:
                f(e)
        build.stats = dict(tk.nins)
    return nc


_W = ("norm_mixer", "gla_w_in", "gla_w_a2", "gla_b_a", "gla_o_norm", "gla_w_out", "fox_w_in", "fox_b_f",
      "fox_q_norm", "fox_k_norm", "fox_w_out", "ple_proj", "ple_gate", "ple_norm")


def kernel(**inputs):
    n = 8
    x = np.ascontiguousarray(inputs["x"], dtype=np.float32)
    p = np.ascontiguousarray(inputs["p"], dtype=np.float32)
    per = x.shape[0] // n
    nc = build(nseq=per)
    shared = {k: np.ascontiguousarray(inputs[k], dtype=np.float32) for k in _W}
    shared["final_norm"] = np.ascontiguousarray(inputs["final_norm"], dtype=np.float32).reshape(1, D)
    in_maps = []
    for c in range(n):
        m = dict(shared)
        m["x"] = np.ascontiguousarray(x[c * per:(c + 1) * per])
        m["p"] = np.ascontiguousarray(p[:, c * per:(c + 1) * per])
        in_maps.append(m)
    res = run_bass_kernel_spmd(nc, in_maps, core_ids=list(range(n)))
    return np.concatenate([r["out"] for r in res.results], axis=0)
```

```python
import numpy as np
from contextlib import ExitStack
import concourse.bass as bass
import concourse.mybir as mybir
from concourse.bass_utils import run_bass_kernel_spmd

F32 = mybir.dt.float32
BF16 = mybir.dt.bfloat16
AF = mybir.ActivationFunctionType
ALU = mybir.AluOpType
AX = mybir.AxisListType

D = 1024
SEQ = 2048
NB = SEQ // 128
EPS = 1e-6


class Tok:
    __slots__ = ("sem", "val", "eng", "dma", "seq", "idx", "used")


class Buf:
    def __init__(self, name):
        self.name = name
        self.w = None
        self.r = {}


class TK:
    EPOCH = 12000

    def __init__(self, nc, st):
        self.nc, self.st = nc, st
        self.ops = {e: [] for e in ("pe", "act", "dve", "pool", "sp")}
        self.waited, self.waited_idx = {}, {}
        self.nsem = 0
        self.seq = 0
        self.nins = {e: 0 for e in self.ops}

    def new_sem(self, name):
        self.nsem += 1
        return self.st.enter_context(self.nc.semaphore(f"{name}_{self.nsem}"))

    def new_dma(self, name):
        return (self.new_sem(name), {"val": 0})

    def op(self, eng, fn, reads=(), writes=(), sig=True, dma=None):
        deps = []
        for b in reads:
            if b.w is not None:
                deps.append(b.w)
        for b in writes:
            if b.w is not None:
                deps.append(b.w)
            for t in b.r.values():
                if (not t.dma) and t.eng == eng:
                    continue
                deps.append(t)
        ops = self.ops[eng]
        n = self.nins[eng]
        for d in deps:
            if d.dma:
                key = (eng, id(d.sem))
                if self.waited.get(key, -1) >= d.val:
                    continue
                self.waited[key] = d.val
                ops.append(("wait", d))
            elif d.eng == eng:
                if eng == "pe" or dma is not None:
                    continue
                key = (eng, d.eng)
                if self.waited_idx.get(key, -1) >= d.idx:
                    continue
                self.waited_idx[key] = d.idx
                d.used = True
                ops.append(("wait", d))
            else:
                key = (eng, d.eng)
                if self.waited_idx.get(key, -1) >= d.idx:
                    continue
                self.waited_idx[key] = d.idx
                d.used = True
                ops.append(("wait", d))
        tok = Tok()
        tok.eng = eng
        tok.dma = dma is not None
        tok.used = False
        tok.sem, tok.val = None, None
        self.seq += 1
        tok.seq = self.seq
        if dma is not None:
            sem, stt = dma
            stt["val"] += 16
            tok.sem, tok.val = sem, stt["val"]
            tok.idx = -1
            ops.append(("dma", fn, tok))
        else:
            tok.idx = n
            self.nins[eng] = n + 1
            ops.append(("op", fn, tok))
        for b in reads:
            b.r[id(tok.sem) if tok.dma else eng] = tok
        for b in writes:
            b.w = tok
            b.r = {}
        return tok

    def raw_wait(self, eng, sem, val):
        self.ops[eng].append(("rawwait", sem, val))

    def finalize(self):
        self.nsig = {}
        for eng, ops in self.ops.items():
            sem, cnt, ns = None, 0, 0
            for it in ops:
                if it[0] == "op" and it[2].used:
                    if sem is None or cnt >= self.EPOCH:
                        sem, cnt = self.new_sem("s_" + eng), 0
                    cnt += 1
                    ns += 1
                    it[2].sem, it[2].val = sem, cnt
            self.nsig[eng] = ns

    def emit(self, eng, e):
        for it in self.ops[eng]:
            if it[0] == "wait":
                e.wait_ge(it[1].sem, it[1].val)
            elif it[0] == "rawwait":
                e.wait_ge(it[1], it[2])
            elif it[0] == "dma":
                it[1](e).then_inc(it[2].sem, 16)
            else:
                ins = it[1](e)
                if it[2].used:
                    ins.then_inc(it[2].sem, 1)

    def alias(self, old, new):
        merged = {}
        for ob in old:
            for t in ([ob.w] if ob.w is not None else []) + list(ob.r.values()):
                k = id(t.sem) if t.dma else t.eng
                if k not in merged or merged[k].seq < t.seq:
                    merged[k] = t
        for nb in new:
            nb.w = None
            nb.r = dict(merged)


class T:
    def __init__(self, ap, name):
        self.ap = ap
        self.b = Buf(name)


def build(nseq=2, layers=(0, 1, 2, 3), final=True, ple=True, gla_nb=NB):
    nc = bass.Bass("TRN2", target_bir_lowering=False)
    dt_in = lambda name, shape: nc.dram_tensor(name, list(shape), F32, kind="ExternalInput").ap()
    x = dt_in("x", [nseq, SEQ, D])
    p = dt_in("p", [4, nseq, SEQ, 256])
    norm_mixer = dt_in("norm_mixer", [4, D])
    gla_w_in = dt_in("gla_w_in", [2, D, 3088])
    gla_w_a2 = dt_in("gla_w_a2", [2, 16, 512])
    gla_b_a = dt_in("gla_b_a", [2, 512])
    gla_o_norm = dt_in("gla_o_norm", [2, 256])
    gla_w_out = dt_in("gla_w_out", [2, D, D])
    fox_w_in = dt_in("fox_w_in", [2, D, 4112])
    fox_b_f = dt_in("fox_b_f", [2, 16])
    fox_q_norm = dt_in("fox_q_norm", [2, 64])
    fox_k_norm = dt_in("fox_k_norm", [2, 64])
    fox_w_out = dt_in("fox_w_out", [2, D, D])
    ple_proj = dt_in("ple_proj", [4, 256, D])
    ple_gate = dt_in("ple_gate", [4, D, D])
    ple_norm = dt_in("ple_norm", [4, D])
    final_norm = dt_in("final_norm", [1, D])
    out = nc.dram_tensor("out", [nseq, SEQ, D], F32, kind="ExternalOutput").ap()

    with ExitStack() as st:
        tk = TK(nc, st)

        def sb(name, shape, dt):
            return st.enter_context(nc.sbuf_tensor(name, list(shape), dt))

        def sbt(name, shape, dt):
            return T(sb(name, shape, dt), name)

        h = sb("h", [128, NB, D], F32)
        hB = [Buf(f"h{b}") for b in range(NB)]
        arena = sb("arena", [128, 33792], BF16)
        arena2 = sb("arena2", [128, 16384], BF16)
        gla_Win = T(arena[:, 0:8 * 3088].rearrange("p (k c) -> p k c", k=8), "gla_Win")
        Wout = T(arena[:, 25600:25600 + 8192].rearrange("p (k c) -> p k c", k=8), "Wout")
        fox_Wg = T(arena[:, 0:8192].rearrange("p (k j c) -> p k j c", k=8, j=4), "fox_Wg")
        fox_KT = T(arena[:, 8192:16384].rearrange("p (h t) -> p h t", h=4), "fox_KT")
        fox_V = T(arena[:, 16384:16384 + 16 * 4 * 65].rearrange("p (b h c) -> p b h c", b=16, h=4), "fox_V")
        fox_Wf = T(arena[:, 20736:20736 + 128].rearrange("p (k c) -> p k c", k=8), "fox_Wf")
        ple_Wg = T(arena[:, 0:8192].rearrange("p (k c) -> p k c", k=8), "ple_Wg")
        ple_Wp = T(arena[:, 8192:10240].rearrange("p (k c) -> p k c", k=2), "ple_Wp")
        KTb = [Buf(f"KT{b}") for b in range(NB)]
        Vb = [Buf(f"V{b}") for b in range(NB)]
        arena_gla = [gla_Win.b]
        arena_fox = [fox_Wg.b, fox_Wf.b] + KTb + Vb
        arena_ple = [ple_Wg.b, ple_Wp.b]
        hT = arena2[:, :].rearrange("p (k t) -> p k t", k=8)
        hTB = [Buf(f"hT{b}") for b in range(NB)]
        a2 = [0]

        def carve(name, n_bf16, dt=BF16):
            ap_ = arena2[:, a2[0]:a2[0] + n_bf16]
            a2[0] += n_bf16
            if dt == F32:
                ap_ = ap_.bitcast(F32)
            return T(ap_, name)

        xe = carve("xe", 1024, F32)
        Lbf = carve("Lbf", 512)
        E_tm = carve("E_tm", 1024, F32)
        Einv_tm = carve("Einv_tm", 1024, F32)
        q_in = carve("q_in", 512)
        k_in = carve("k_in", 512)
        q_inT = carve("q_inT", 512)
        k_inT = carve("k_inT", 512)
        v_sb = carve("v_sb", 1024)
        attnT = carve("attnT", 512)
        S_f32 = carve("S_f32", 2048, F32)
        S_bf = carve("S_bf", 1024)
        k_in_b = carve("k_in_b", 512)
        q_inT_b = carve("q_inT_b", 512)
        k_inT_b = carve("k_inT_b", 512)
        v_sb_b = carve("v_sb_b", 1024)
        k_in2, q_inT2, k_inT2, v_sb2 = [k_in, k_in_b], [q_inT, q_inT_b], [k_inT, k_inT_b], [v_sb, v_sb_b]
        gla_work = [xe, Lbf, E_tm, Einv_tm, q_in, k_in, q_inT, k_inT, v_sb, attnT, S_f32, S_bf, k_in_b, q_inT_b, k_inT_b, v_sb_b]
        assert a2[0] <= 16384

        gbc_m = sbt("gbc_m", [128, D], F32)
        gbc_p = sbt("gbc_p", [128, D], F32)
        hb = [sbt(f"hb{i}", [128, D], BF16) for i in range(2)]
        sqj = sbt("sqj", [128, D], BF16)
        hTb = [sbt(f"hTb{i}", [128, 8, 128], BF16) for i in range(2)]
        e_g = sbt("e_g", [128, D], F32)
        sg = sbt("sg", [128, D], F32)
        sgF = []
        for i_ in range(2):
            t_ = T(sg.ap[:, i_ * 256:(i_ + 1) * 256], f"sgF{i_}")
            t_.b = sg.b
            sgF.append(t_)
        og = sbt("og", [128, D], BF16)
        ogT = sbt("ogT", [128, 8, 128], BF16)
        p_b = [sbt(f"p_b{i}", [128, 256], BF16) for i in range(2)]
        pT2 = [sbt(f"pT{i}", [128, 2, 128], BF16) for i in range(2)]
        stats = sbt("stats", [128, 64], F32)
        rstd_all = sbt("rstd_all", [128, NB], F32)
        ident = sbt("ident", [128, 128], BF16)
        maskT = sbt("maskT", [128, 128], BF16)
        tri32 = sbt("tri32", [128, 128], F32)
        ones32 = sbt("ones32", [128, 128], F32)
        ones_bf = sbt("ones_bf", [128, 1], BF16)
        a_sb = sbt("a_sb", [128, 16], BF16)
        aT = sbt("aT", [17, 128], BF16)
        wa2 = sbt("wa2", [17, 512], BF16)
        gcol = sbt("gcol", [128, 2], F32)
        decay2 = [sbt(f"decay{i}", [128, 4], F32) for i in range(2)]
        decay = decay2[0]
        sso = sbt("sso", [128, 8], F32)
        kq_aug = [sbt(f"kq_aug{i}", [128, 4, 68], BF16) for i in range(2)]
        qT2 = [sbt(f"qT{i}", [68, 4, 128], BF16) for i in range(2)]
        af = [20864]

        def carve_fox(name, n_bf16, dt=BF16):
            ap_ = arena[:, af[0]:af[0] + n_bf16]
            af[0] += n_bf16
            if dt == F32:
                ap_ = ap_.bitcast(F32)
            return T(ap_, name)

        PT = [T(carve_fox(f"PT{i}", 512).ap.rearrange("p (j t) -> p j t", j=4), f"PT{i}") for i in range(3)]
        sqk2 = [carve_fox(f"sqk{i}", 512, F32) for i in range(2)]
        fst2 = [sbt(f"fst{i}", [128, 64], F32) for i in range(2)]
        kgain = sbt("kgain", [128, 64], F32)
        qgain = sbt("qgain", [128, 64], F32)
        bf_bc = sbt("bf_bc", [128, 16], F32)
        Lf = sbt("Lf", [128, 16], F32)
        Rf = sbt("Rf", [128, 16], F32)
        C_all = T(carve_fox("C_all", 512, F32).ap.rearrange("p (b c) -> p b c", b=NB), "C_all")
        Chl = T(carve_fox("Chl", 512).ap.rearrange("p (b a c) -> p b a c", b=NB, a=2), "Chl")
        Ctmp = sbt("Ctmp", [128, 16], F32)
        CB = [Buf(f"C{b}") for b in range(NB)]
        rden = sbt("rden", [128, 4], F32)
        otmp = carve_fox("otmp", 512, F32)
        assert af[0] <= 25600
        arena_fox += [t.b for t in PT + sqk2 + [otmp]] + CB

        psF = [T(st.enter_context(nc.psum_tensor(f"psF{i}", [128, 512], F32)), f"psF{i}") for i in range(6)]
        psB = [T(st.enter_context(nc.psum_tensor(f"psB{i}", [128, 1024], BF16)), f"psB{i}") for i in range(2)]
        rr = {"F": 0, "B": 0, "hb": 0, "hTb": 0, "pb": 0, "PT": 0, "aug": 0}

        def nextF(n=5):
            rr["F"] = (rr["F"] + 1) % n
            return psF[rr["F"]]

        def nextB():
            rr["B"] ^= 1
            return psB[rr["B"]]

        def rot(key, lst):
            rr[key] = (rr[key] + 1) % len(lst)
            return lst[rr[key]]

        def bl(xs):
            return [t.b if isinstance(t, T) else t for t in xs]

        def mm(o, lhsT, rhs, start, stop, reads, writes, sig=None):
            tk.op("pe", lambda e: e.matmul(o, lhsT=lhsT, rhs=rhs, start=start, stop=stop),
                  bl(reads), bl(writes), sig=(stop if sig is None else sig))

        def tr(o, i, reads, writes, sig=True):
            tk.op("pe", lambda e: e.transpose(out=o, in_=i, identity=ident.ap[:]), bl(reads) + [ident.b], bl(writes), sig=sig)

        def act(o, i, func, reads, writes, bias=None, scale=None, accum=None):
            kw = {}
            if bias is not None:
                kw["bias"] = bias
            if scale is not None:
                kw["scale"] = scale
            if accum is not None:
                kw["accum_out"] = accum
            tk.op("act", lambda e: e.activation(out=o, in_=i, func=func, **kw), bl(reads), bl(writes))

        def dve(fn, reads, writes):
            tk.op("dve", fn, bl(reads), bl(writes))

        def tt(o, a, b_, op, reads, writes):
            dve(lambda e: e.tensor_tensor(out=o, in0=a, in1=b_, op=op), reads, writes)

        def ts(o, a, s1, s2, op0, op1, reads, writes):
            if s2 is None:
                dve(lambda e: e.tensor_scalar(out=o, in0=a, scalar1=s1, scalar2=None, op0=op0), reads, writes)
            else:
                dve(lambda e: e.tensor_scalar(out=o, in0=a, scalar1=s1, scalar2=s2, op0=op0, op1=op1), reads, writes)

        def stt(o, a, s, b_, op0, op1, reads, writes):
            dve(lambda e: e.scalar_tensor_tensor(out=o, in0=a, scalar=s, in1=b_, op0=op0, op1=op1), reads, writes)

        def cp(o, i, reads, writes):
            dve(lambda e: e.tensor_copy(out=o, in_=i), reads, writes)

        def wload(o, i, writes, dmah):
            tk.op("pool", lambda e: e.dma_start(out=o, in_=i), [], bl(writes), dma=dmah)

        def wload_nc(o, i, writes, dmah):
            tk.op("pool", lambda e: e.dma_start(out=o, in_=i, allow_slow_non_contiguous=True), [], bl(writes), dma=dmah)

        dm = {}

        def dmah(name):
            if name not in dm:
                dm[name] = tk.new_dma(name)
            return dm[name]

        pl = lambda fn, reads, writes: tk.op("dve", fn, bl(reads), bl(writes))

        def pool(fn, reads, writes):
            tk.op("pool", fn, bl(reads), bl(writes))

        pool(lambda e: e.memset(ones32.ap[:], 1.0), [], [ones32])
        pool(lambda e: e.memset(tri32.ap[:], 1.0), [], [tri32])
        pool(lambda e: e.affine_select(out=tri32.ap[:], in_=tri32.ap[:], pattern=[[1, 128]], compare_op=ALU.is_ge,
                                       fill=0.0, base=0, channel_multiplier=-1), [tri32], [tri32])
        cfv = e_g.ap[:, 0:128]
        pool(lambda e: e.memset(cfv, 1.0), [], [e_g])
        pool(lambda e: e.affine_select(out=cfv, in_=cfv, pattern=[[-1, 128]], compare_op=ALU.is_equal,
                                       fill=0.0, base=0, channel_multiplier=1), [e_g], [e_g])
        cp(ident.ap[:], cfv, [e_g], [ident])
        cp(maskT.ap[:], tri32.ap[:], [tri32], [maskT])
        cp(ones_bf.ap[:], ones32.ap[:, 0:1], [ones32], [ones_bf])
        dve(lambda e: e.memset(aT.ap[:], 1.0), [], [aT])

        def pass0(b, gbc, rstd_ap, hT_dst, hT_buf):
            hbt = rot("hb", hb)
            act(sqj.ap[:], h[:, b, :], AF.Square, [hB[b]], [sqj, stats], accum=stats.ap[:, 0:1])
            act(stats.ap[:, 1:2], stats.ap[:, 0:1], AF.Ln, [stats], [stats], bias=EPS, scale=1.0 / D)
            act(rstd_ap, stats.ap[:, 1:2], AF.Exp, [stats], [rstd_all], scale=-0.5)
            tt(hbt.ap[:], h[:, b, :], gbc.ap[:], ALU.mult, [hB[b], gbc], [hbt])
            pb_ = nextB()
            for k in range(8):
                tr(pb_.ap[:, k * 128:(k + 1) * 128], hbt.ap[:, k * 128:(k + 1) * 128], [hbt], [pb_], sig=(k == 7))
            cp(hT_dst, pb_.ap[:, :].rearrange("p (k t) -> p k t", k=8), [pb_], [hT_buf])

        def load_gbc(dst, src_row):
            wload(dst.ap[:], src_row.to_broadcast([128, D]), [dst], dmah(dst.b.name))

        def proj(hT_ap, hT_buf, W_ap_fn, ncols, Wbuf, bank):
            for k in range(8):
                mm(bank.ap[:, 0:ncols], hT_ap[:, k, :], W_ap_fn(k), k == 0, k == 7, [hT_buf, Wbuf], [bank])

        def sigmoid_act(dst, src, nrs_ap, reads, wbuf):
            act(dst, src, AF.Exp, reads, [wbuf], scale=nrs_ap)
            act(dst, dst, AF.Ln, [wbuf], [wbuf], bias=1.0)
            act(dst, dst, AF.Exp, [wbuf], [wbuf], scale=-1.0)

        def ple_pass(i, s):
            tk.alias(arena_gla + arena_fox, arena_ple)
            wg_view = ple_gate[i].rearrange("(k p) c -> p k c", p=128)
            for k in range(8):
                wload(ple_Wg.ap[:, k, :], wg_view[:, k, :], [ple_Wg], dmah("ple_Wg"))
            wload(ple_Wp.ap[:, :, :], ple_proj[i].rearrange("(k p) c -> p k c", p=128), [ple_Wp], dmah("ple_Wp"))
            load_gbc(gbc_p, ple_norm[i:i + 1, :])

            def stA(b):
                rs = rstd_all.ap[:, b:b + 1]
                hTt = hTb[b % 2]
                pbt = p_b[b % 2]
                pTt = pT2[b % 2]
                wload(pbt.ap[:], p[i, s, b * 128:(b + 1) * 128, :], [pbt], dmah(pbt.b.name))
                pass0(b, gbc_p, rs, hTt.ap[:], hTt.b)
                pb_ = nextB()
                for k in range(2):
                    tr(pb_.ap[:, k * 128:(k + 1) * 128], pbt.ap[:, k * 128:(k + 1) * 128], [pbt], [pb_], sig=(k == 1))
                cp(pTt.ap[:], pb_.ap[:, 0:256].rearrange("p (k t) -> p k t", k=2), [pb_], [pTt])

            def stB(b):
                rs = rstd_all.ap[:, b:b + 1]
                hTt = hTb[b % 2]
                pTt = pT2[b % 2]
                ts(stats.ap[:, 2:3], rs, -1.0, None, ALU.mult, None, [rstd_all], [stats])
                for cg in range(2):
                    cs = slice(cg * 512, (cg + 1) * 512)
                    bg = nextF()
                    proj(hTt.ap, hTt.b, lambda k: ple_Wg.ap[:, k, cs], 512, ple_Wg.b, bg)
                    sigmoid_act(sg.ap[:, cs], bg.ap[:], stats.ap[:, 2:3], [bg, stats], sg)
                    bp = nextF()
                    for k in range(2):
                        mm(bp.ap[:], pTt.ap[:, k, :], ple_Wp.ap[:, k, cs], k == 0, k == 1, [pTt, ple_Wp], [bp])
                    tt(e_g.ap[:, cs], bp.ap[:], sg.ap[:, cs], ALU.mult, [bp, sg], [e_g])
                    tt(h[:, b, cs], h[:, b, cs], e_g.ap[:, cs], ALU.add, [hB[b], e_g], [hB[b]])

            stA(0)
            for b in range(NB):
                if b + 1 < NB:
                    stA(b + 1)
                stB(b)

        def gla_layer(i, s):
            j = i // 2
            tk.alias(arena_ple + arena_fox, arena_gla)
            tk.alias(hTB, [t.b for t in gla_work])
            wv = gla_w_in[j].rearrange("(k p) c -> p k c", p=128)
            for (c0, c1) in ((3072, 3088), (0, 512), (512, 1024), (1024, 1536), (1536, 2048), (2048, 2560), (2560, 3072)):
                for k0 in (0, 4):
                    wload(gla_Win.ap[:, k0:k0 + 4, c0:c1], wv[:, k0:k0 + 4, c0:c1], [gla_Win], dmah("gla_Win"))
            wo = gla_w_out[j].rearrange("(k p) c -> p k c", p=128)
            for k0 in (0, 4):
                wload(Wout.ap[:, k0:k0 + 4, :], wo[:, k0:k0 + 4, :], [Wout], dmah("Wout"))
            wload(wa2.ap[0:16, :], gla_w_a2[j], [wa2], dmah("wa2"))
            wload(wa2.ap[16:17, :], gla_b_a[j:j + 1, :], [wa2], dmah("wa2"))
            tk.op("sp", lambda e: e.dma_start(out=gcol.ap[:], in_=gla_o_norm[j].rearrange("(a p) -> p a", p=128),
                                              allow_slow_non_contiguous=True), [], [gcol.b], dma=dmah("gcol"))
            load_gbc(gbc_m, norm_mixer[i:i + 1, :])
            for k in range(8):
                ts(Wout.ap[:, k, :], Wout.ap[:, k, :], gcol.ap[:, (k % 2):(k % 2) + 1], None, ALU.mult, None, [Wout, gcol], [Wout])
            Win = gla_Win
            sg2 = [sg, e_g]

            def stA(b):
                par = b % 2
                rs = rstd_all.ap[:, b:b + 1]
                hTt = hTb[par]
                k_in_, q_inT_, k_inT_, v_, decay, sgt = k_in2[par], q_inT2[par], k_inT2[par], v_sb2[par], decay2[par], sg2[par]
                pass0(b, gbc_m, rs, hTt.ap[:], hTt.b)
                ba = nextF()
                proj(hTt.ap, hTt.b, lambda k: Win.ap[:, k, 3072:3088], 16, Win.b, ba)
                act(a_sb.ap[:], ba.ap[:, 0:16], AF.Copy, [ba, rstd_all], [a_sb], scale=rs)
                pb_ = nextB()
                tr(pb_.ap[0:16, 0:128], a_sb.ap[:], [a_sb], [pb_])
                cp(aT.ap[0:16, :], pb_.ap[0:16, 0:128], [pb_], [aT])
                for cg in range(2):
                    bv = nextF()
                    proj(hTt.ap, hTt.b, lambda k: Win.ap[:, k, 1024 + cg * 512:1536 + cg * 512], 512, Win.b, bv)
                    act(v_.ap[:, cg * 512:(cg + 1) * 512], bv.ap[:], AF.Copy, [bv, rstd_all], [v_], scale=rs)
                bx = nextF()
                mm(bx.ap[:], aT.ap[:], wa2.ap[:], True, True, [aT, wa2], [bx])
                act(xe.ap[:], bx.ap[:], AF.Exp, [bx], [xe], scale=-1.0)
                act(xe.ap[:], xe.ap[:], AF.Ln, [xe], [xe], bias=1.0)
                cp(Lbf.ap[:], xe.ap[:], [xe], [Lbf])
                bc = nextF()
                mm(bc.ap[:], maskT.ap[:], Lbf.ap[:], True, True, [maskT, Lbf], [bc])
                act(E_tm.ap[:], bc.ap[:], AF.Exp, [bc], [E_tm], scale=-1.0 / 16, bias=float(np.log(128.0 ** -0.5)))
                act(Einv_tm.ap[:], bc.ap[:], AF.Exp, [bc], [Einv_tm], scale=1.0 / 16)
                bd = nextF()
                for hh in range(4):
                    mm(bd.ap[:, hh:hh + 1], Lbf.ap[:, hh * 128:(hh + 1) * 128], ones_bf.ap[:], True, True, [Lbf, ones_bf], [bd], sig=(hh == 3))
                act(decay.ap[:], bd.ap[:, 0:4], AF.Exp, [bd], [decay], scale=-1.0 / 16)
                bq = nextF()
                proj(hTt.ap, hTt.b, lambda k: Win.ap[:, k, 0:512], 512, Win.b, bq)
                stt(q_in.ap[:], bq.ap[:], rs, E_tm.ap[:], ALU.mult, ALU.mult, [bq, rstd_all, E_tm], [q_in])
                bk_ = nextF()
                proj(hTt.ap, hTt.b, lambda k: Win.ap[:, k, 512:1024], 512, Win.b, bk_)
                stt(k_in_.ap[:], bk_.ap[:], rs, Einv_tm.ap[:], ALU.mult, ALU.mult, [bk_, rstd_all, Einv_tm], [k_in_])
                for (src, dst) in ((q_in, q_inT_), (k_in_, k_inT_)):
                    pb_ = nextB()
                    for hh in range(4):
                        tr(pb_.ap[:, hh * 128:(hh + 1) * 128], src.ap[:, hh * 128:(hh + 1) * 128], [src], [pb_], sig=(hh == 3))
                    cp(dst.ap[:], pb_.ap[:, 0:512], [pb_], [dst])
                for cg in range(2):
                    cs = slice(cg * 512, (cg + 1) * 512)
                    bg = nextF()
                    proj(hTt.ap, hTt.b, lambda k: Win.ap[:, k, 2048 + cg * 512:2560 + cg * 512], 512, Win.b, bg)
                    act(sgt.ap[:, cs], bg.ap[:], AF.Silu, [bg, rstd_all], [sgt], scale=rs)

            def stB(b):
                par = b % 2
                k_in_, q_inT_, k_inT_, v_, decay, sgt = k_in2[par], q_inT2[par], k_inT2[par], v_sb2[par], decay2[par], sg2[par]
                bt = nextF()
                for hh in range(4):
                    mm(bt.ap[:, hh * 128:(hh + 1) * 128], k_inT_.ap[:, hh * 128:(hh + 1) * 128], q_inT_.ap[:, hh * 128:(hh + 1) * 128],
                       True, True, [k_inT_, q_inT_], [bt], sig=(hh == 3))
                tt(attnT.ap[:].rearrange("p (h c) -> p h c", h=4), bt.ap[:].rearrange("p (h c) -> p h c", h=4),
                   maskT.ap[:].unsqueeze(1).to_broadcast([128, 4, 128]), ALU.mult, [bt, maskT], [attnT])
                bo = [nextF(), nextF()]
                for hh in range(4):
                    o_ap = bo[hh // 2].ap[:, (hh % 2) * 256:(hh % 2) * 256 + 256]
                    mm(o_ap, attnT.ap[:, hh * 128:(hh + 1) * 128], v_.ap[:, hh * 256:(hh + 1) * 256], True, b == 0,
                       [attnT, v_], [bo[hh // 2]], sig=(b == 0 and hh % 2 == 1))
                    if b > 0:
                        mm(o_ap, q_inT_.ap[:, hh * 128:(hh + 1) * 128], S_bf.ap[:, hh * 256:(hh + 1) * 256], False, True,
                           [q_inT_, S_bf], [bo[hh // 2]], sig=(hh % 2 == 1))
                bss = []
                if b < NB - 1:
                    for half in range(2):
                        bs = nextF()
                        bss.append(bs)
                        for hq in range(2):
                            hh = half * 2 + hq
                            mm(bs.ap[:, hq * 256:(hq + 1) * 256], k_in_.ap[:, hh * 128:(hh + 1) * 128], v_.ap[:, hh * 256:(hh + 1) * 256],
                               True, True, [k_in_, v_], [bs], sig=(hq == 1))
                for hh in range(4):
                    o_ap = bo[hh // 2].ap[:, (hh % 2) * 256:(hh % 2) * 256 + 256]
                    act(sqj.ap[:, 0:256], o_ap, AF.Square, [bo[hh // 2]], [sqj, sso], accum=sso.ap[:, hh:hh + 1])
                act(sso.ap[:, 4:8], sso.ap[:, 0:4], AF.Ln, [sso], [sso], bias=EPS, scale=1.0 / 256)
                act(sso.ap[:, 0:4], sso.ap[:, 4:8], AF.Exp, [sso], [sso], scale=-0.5)
                for hh in range(4):
                    o_ap = bo[hh // 2].ap[:, (hh % 2) * 256:(hh % 2) * 256 + 256]
                    stt(og.ap[:, hh * 256:(hh + 1) * 256], o_ap, sso.ap[:, hh:hh + 1], sgt.ap[:, hh * 256:(hh + 1) * 256],
                        ALU.mult, ALU.mult, [bo[hh // 2], sso, sgt], [og])
                out_proj(b, range(8))
                if b < NB - 1:
                    for half in range(2):
                        bs = bss[half]
                        Sv = S_f32.ap[:, half * 512:(half + 1) * 512]
                        if b == 0:
                            cp(Sv, bs.ap[:], [bs], [S_f32])
                        else:
                            tt(Sv, Sv, bs.ap[:], ALU.add, [bs, S_f32], [S_f32])
                        for hq in range(2):
                            hh = half * 2 + hq
                            Sh = S_f32.ap[:, hh * 256:(hh + 1) * 256]
                            ts(Sh, Sh, decay.ap[:, hh:hh + 1], None, ALU.mult, None, [S_f32, decay], [S_f32])
                    act(S_bf.ap[:], S_f32.ap[:], AF.Copy, [S_f32], [S_bf])

            stA(0)
            for b in range(gla_nb):
                if b + 1 < gla_nb:
                    stA(b + 1)
                stB(b)

        def out_proj(b, chunks, nf=5):
            chunks = list(chunks)
            n = len(chunks)
            pb_ = nextB()
            for ci, k in enumerate(chunks):
                tr(pb_.ap[:, ci * 128:(ci + 1) * 128], og.ap[:, ci * 128:(ci + 1) * 128], [og], [pb_], sig=(ci == n - 1))
            dve(lambda e: e.tensor_copy(out=ogT.ap[:, 0:n, :], in_=pb_.ap[:, 0:n * 128].rearrange("p (k t) -> p k t", k=n)), [pb_], [ogT])
            for cg in range(2):
                cs = slice(cg * 512, (cg + 1) * 512)
                by = nextF(nf)
                for ci, k in enumerate(chunks):
                    mm(by.ap[:], ogT.ap[:, ci, :], Wout.ap[:, k, cs], ci == 0, ci == n - 1, [ogT, Wout], [by])
                tt(h[:, b, cs], h[:, b, cs], by.ap[:], ALU.add, [hB[b], by], [hB[b]])

        def fox_layer(i, s):
            j = i // 2
            tk.alias(arena_ple + arena_gla, arena_fox)
            tk.alias([t.b for t in gla_work], hTB)
            wv = fox_w_in[j].rearrange("(k p) c -> p k c", p=128)
            wo = fox_w_out[j].rearrange("(k p) c -> p k c", p=128)
            for k0 in (0, 4):
                wload(Wout.ap[:, k0:k0 + 4, :], wo[:, k0:k0 + 4, :], [Wout], dmah("Wout"))
            wload(fox_Wf.ap[:, :, :], wv[:, :, 4096:4112], [fox_Wf], dmah("fox_Wf"))
            load_gbc(gbc_m, norm_mixer[i:i + 1, :])
            wload(kgain.ap[:], fox_k_norm[j:j + 1, :].to_broadcast([128, 64]), [kgain], dmah("kgain"))
            wload(qgain.ap[:], fox_q_norm[j:j + 1, :].to_broadcast([128, 64]), [qgain], dmah("qgain"))
            wload(bf_bc.ap[:], fox_b_f[j:j + 1, :].to_broadcast([128, 16]), [bf_bc], dmah("bf_bc"))
            ts(qgain.ap[:], qgain.ap[:], 0.125, None, ALU.mult, None, [qgain], [qgain])
            COLB = (0, 3072, 1024, 2048)

            def load_group(G):
                for jj in range(4):
                    c0 = COLB[jj] + G * 256
                    wload(fox_Wg.ap[:, :, jj, :], wv[:, :, c0:c0 + 256], [fox_Wg], dmah("fox_Wg"))

            load_group(0)
            dve(lambda e: e.memset(Rf.ap[:], 0.0), [], [Rf])
            for b in range(NB):
                rs = rstd_all.ap[:, b:b + 1]
                pass0(b, gbc_m, rs, hT[:, :, b * 128:(b + 1) * 128], hTB[b])
                bf_ = nextF(4)
                proj(hT[:, :, b * 128:(b + 1) * 128], hTB[b], lambda k: fox_Wf.ap[:, k, :], 16, fox_Wf.b, bf_)
                stt(Lf.ap[:], bf_.ap[:, 0:16], rs, bf_bc.ap[:], ALU.mult, ALU.add, [bf_, rstd_all, bf_bc], [Lf])
                act(Lf.ap[:], Lf.ap[:], AF.Exp, [Lf], [Lf], scale=-1.0)
                act(Lf.ap[:], Lf.ap[:], AF.Ln, [Lf], [Lf], bias=1.0)
                bc = nextF(4)
                mm(bc.ap[:, 0:16], tri32.ap[:], Lf.ap[:], True, False, [tri32, Lf], [bc], sig=False)
                mm(bc.ap[:, 0:16], ones32.ap[:], Rf.ap[:], False, True, [ones32, Rf], [bc])
                cp(C_all.ap[:, b, :], bc.ap[:, 0:16], [bc], [CB[b]])
                tt(Rf.ap[:], Rf.ap[:], Lf.ap[:], ALU.add, [Rf, Lf], [Rf])
                cp(Chl.ap[:, b, 0, :], C_all.ap[:, b, :], [CB[b]], [CB[b]])
                tt(Ctmp.ap[:], C_all.ap[:, b, :], Chl.ap[:, b, 0, :], ALU.subtract, [CB[b]], [Ctmp])
                cp(Chl.ap[:, b, 1, :], Ctmp.ap[:], [Ctmp], [CB[b]])
            dve(lambda e: e.memset(fox_V.ap[:, :, :, 64:65], 1.0), [], Vb)

            def qk_norm_aug(bank, rs, b, G, gain, is_q, aug, par):
                sqk = sqk2[par]
                fst = fst2[par]
                act(sqk.ap[:], bank.ap[:, 0:256], AF.Square, [bank], [sqk])
                yield
                dve(lambda e: e.reduce_sum(out=fst.ap[:, 0:4], in_=sqk.ap[:].rearrange("p (h d) -> p h d", h=4), axis=AX.X), [sqk], [fst])
                tt(fst.ap[:, 8:9], rs, rs, ALU.mult, [rstd_all], [fst])
                ts(fst.ap[:, 4:8], fst.ap[:, 0:4], fst.ap[:, 8:9], None, ALU.mult, None, [fst], [fst])
                yield
                act(fst.ap[:, 12:16], fst.ap[:, 4:8], AF.Ln, [fst], [fst], bias=EPS, scale=1.0 / 64)
                act(fst.ap[:, 16:20], fst.ap[:, 12:16], AF.Exp, [fst], [fst], scale=-0.5)
                yield
                ts(fst.ap[:, 20:24], fst.ap[:, 16:20], rs, None, ALU.mult, None, [fst, rstd_all], [fst])
                tt(sqk.ap[:].rearrange("p (h d) -> p h d", h=4), bank.ap[:, 0:256].rearrange("p (h d) -> p h d", h=4),
                   fst.ap[:, 20:24].unsqueeze(2).to_broadcast([128, 4, 64]), ALU.mult, [bank, fst], [sqk])
                tt(aug.ap[:, :, 0:64], sqk.ap[:].rearrange("p (h d) -> p h d", h=4),
                   gain.ap[:].unsqueeze(1).to_broadcast([128, 4, 64]), ALU.mult, [sqk, gain], [aug])
                hs = slice(G * 4, G * 4 + 4)
                if is_q:
                    ts(aug.ap[:, :, 64:66], Chl.ap[:, b, :, hs].rearrange("p a h -> p h a"), -1.0, None, ALU.mult, None, [CB[b]], [aug])
                    dve(lambda e: e.memset(aug.ap[:, :, 66:68], 1.0), [], [aug])
                else:
                    dve(lambda e: e.memset(aug.ap[:, :, 64:66], 1.0), [], [aug])
                    cp(aug.ap[:, :, 66:68], Chl.ap[:, b, :, hs].rearrange("p a h -> p h a"), [CB[b]], [aug])
                yield

            for G in range(4):
                if G > 0:
                    load_group(G)

                def p1A(b):
                    rs = rstd_all.ap[:, b:b + 1]
                    hTs = hT[:, :, b * 128:(b + 1) * 128]
                    bk_ = nextF(4)
                    proj(hTs, hTB[b], lambda k: fox_Wg.ap[:, k, 2:4, :], 512, fox_Wg.b, bk_)
                    for _ in qk_norm_aug(bk_, rs, b, G, kgain, False, kq_aug[b % 2], b % 2):
                        pass
                    act(fox_V.ap[:, b, :, 0:64], bk_.ap[:, 256:512].rearrange("p (h d) -> p h d", h=4), AF.Copy, [bk_, rstd_all], [Vb[b]], scale=rs)

                def p1B(b):
                    aug = kq_aug[b % 2]
                    pb_ = nextB()
                    for hh in range(4):
                        tr(pb_.ap[0:68, hh * 128:(hh + 1) * 128], aug.ap[:, hh, :], [aug], [pb_], sig=(hh == 3))
                    cp(fox_KT.ap[0:68, :, b * 128:(b + 1) * 128], pb_.ap[0:68, 0:512].rearrange("p (h t) -> p h t", h=4), [pb_], [KTb[b]])

                p1A(0)
                for b in range(NB):
                    if b + 1 < NB:
                        p1A(b + 1)
                    p1B(b)

                def front(b):
                    rs = rstd_all.ap[:, b:b + 1]
                    hTs = hT[:, :, b * 128:(b + 1) * 128]
                    bq = psF[3]
                    proj(hTs, hTB[b], lambda k: fox_Wg.ap[:, k, 0:2, :], 512, fox_Wg.b, bq)
                    yield
                    yield from qk_norm_aug(bq, rs, b, G, qgain, True, kq_aug[b % 2], b % 2)
                    ts(stats.ap[:, 2:3], rs, -1.0, None, ALU.mult, None, [rstd_all], [stats])
                    dst = e_g.ap[:, 0:256]
                    act(dst, bq.ap[:, 256:512], AF.Exp, [bq, stats], [e_g], scale=stats.ap[:, 2:3])
                    yield
                    act(dst, dst, AF.Ln, [e_g], [e_g], bias=1.0)
                    yield
                    act(dst, dst, AF.Exp, [e_g], [e_g], scale=-1.0)
                    yield
                    sgt = sgF[b % 2]
                    stt(sgt.ap[:], bq.ap[:, 256:512], rs, e_g.ap[:, 0:256], ALU.mult, ALU.mult, [bq, rstd_all, e_g], [sgt])

                def frontB(b):
                    aug = kq_aug[b % 2]
                    qTt = qT2[b % 2]
                    pb_ = nextB()
                    for hh in range(4):
                        tr(pb_.ap[0:68, hh * 128:(hh + 1) * 128], aug.ap[:, hh, :], [aug], [pb_], sig=(hh == 3))
                    cp(qTt.ap[:], pb_.ap[0:68, 0:512].rearrange("p (h t) -> p h t", h=4), [pb_], [qTt])

                def attn(b, filler):
                    qTt = qT2[b % 2]
                    oacc = psF[4 + (b % 2)]
                    banks = {}

                    def S(jb):
                        bs = nextF(3)
                        banks[jb] = bs
                        for hh in range(4):
                            mm(bs.ap[:, hh * 128:(hh + 1) * 128], fox_KT.ap[0:68, hh, jb * 128:(jb + 1) * 128], qTt.ap[:, hh, :],
                               True, True, [KTb[jb], qTt], [bs], sig=(hh == 3))

                    def P(jb):
                        bs = banks.pop(jb)
                        pt = rot("PT", PT)
                        act(pt.ap[:], bs.ap[:].rearrange("p (j t) -> p j t", j=4), AF.Exp, [bs], [pt])
                        if jb == b:
                            tt(pt.ap[:], pt.ap[:], maskT.ap[:].unsqueeze(1).to_broadcast([128, 4, 128]), ALU.mult, [pt, maskT], [pt])
                        for hh in range(4):
                            mm(oacc.ap[:, hh * 65:(hh + 1) * 65], pt.ap[:, hh, :], fox_V.ap[:, jb, hh, :], (jb == 0 and hh == 0), (jb == b and hh == 3),
                               [pt, Vb[jb]], [oacc], sig=(hh == 3))

                    LA = 2
                    for jb in range(min(LA, b + 1)):
                        S(jb)
                    for jb in range(b + 1):
                        if jb + LA <= b:
                            S(jb + LA)
                        P(jb)
                        if filler is not None:
                            next(filler, None)
                    if filler is not None:
                        for _ in filler:
                            pass

                def backD(b):
                    oacc = psF[4 + (b % 2)]
                    o3 = oacc.ap[:, 0:260].rearrange("p (h c) -> p h c", h=4)
                    dve(lambda e: e.reciprocal(out=rden.ap[:].unsqueeze(2), in_=o3[:, :, 64:65]), [oacc], [rden])
                    tt(otmp.ap[:].rearrange("p (h d) -> p h d", h=4), o3[:, :, 0:64],
                       rden.ap[:].unsqueeze(2).to_broadcast([128, 4, 64]), ALU.mult, [oacc, rden], [otmp])
                    tt(og.ap[:, 0:256], otmp.ap[:], sgF[b % 2].ap[:], ALU.mult, [otmp, sgF[b % 2]], [og])

                def backP(b):
                    out_proj(b, [2 * G, 2 * G + 1], nf=4)

                for _ in front(0):
                    pass
                frontB(0)
                for b in range(NB):
                    if b >= 1:
                        backD(b - 1)
                    attn(b, front(b + 1) if b + 1 < NB else None)
                    if b + 1 < NB:
                        frontB(b + 1)
                    if b >= 1:
                        backP(b - 1)
                backD(NB - 1)
                backP(NB - 1)

        def final_pass(s):
            load_gbc(gbc_m, final_norm[0:1, :])
            stg = [e_g, sg]
            for b in range(NB):
                act(sqj.ap[:], h[:, b, :], AF.Square, [hB[b]], [sqj, stats], accum=stats.ap[:, 0:1])
                act(stats.ap[:, 1:2], stats.ap[:, 0:1], AF.Ln, [stats], [stats], bias=EPS, scale=1.0 / D)
                act(stats.ap[:, 3:4], stats.ap[:, 1:2], AF.Exp, [stats], [stats], scale=-0.5)
                t_ = stg[b % 2]
                stt(t_.ap[:], h[:, b, :], stats.ap[:, 3:4], gbc_m.ap[:], ALU.mult, ALU.mult, [hB[b], stats, gbc_m], [t_])
                tk.op("sp", lambda e, t_=t_, b=b: e.dma_start(out=out[s, b * 128:(b + 1) * 128, :], in_=t_.ap[:]),
                      [t_.b], [], dma=dmah(f"st{b % 2}"))

        def raw_store(s):
            for b in range(NB):
                tk.op("sp", lambda e, b=b: e.dma_start(out=out[s, b * 128:(b + 1) * 128, :], in_=h[:, b, :]),
                      [hB[b]], [], dma=dmah(f"st{b % 2}"))

        for s in range(nseq):
            for b in range(NB):
                tk.op("sp", lambda e, b=b, s=s: e.dma_start(out=h[:, b, :], in_=x[s, b * 128:(b + 1) * 128, :]), [], [hB[b]], dma=dmah("xload"))
            last = hB[NB - 1].w
            for b in range(NB):
                hB[b].w = last
            for i in layers:
                if i % 2 == 0:
                    gla_layer(i, s)
                else:
                    fox_layer(i, s)
                if ple:
                    ple_pass(i, s)
            if final:
                final_pass(s)
            else:
                raw_store(s)
        for name in ("st0", "st1"):
            sem, stt_ = dmah(name)
            tk.raw_wait("sp", sem, stt_["val"])
        tk.finalize()

        block = st.enter_context(nc.Block())

        @block.tensor
        def _(e):
            tk.emit("pe", e)

        @block.scalar
        def _(e):
            tk.emit("act", e)

        @block.vector
        def _(e):
            tk.emit("dve", e)

        @block.gpsimd
        def _(e):
            tk.emit("pool", e)

        @block.sync
        def _(e):
            tk.emit("sp", e)
        build.stats = (dict(tk.nins), dict(tk.nsig))
    return nc


_W = ("norm_mixer", "gla_w_in", "gla_w_a2", "gla_b_a", "gla_o_norm", "gla_w_out", "fox_w_in", "fox_b_f",
      "fox_q_norm", "fox_k_norm", "fox_w_out", "ple_proj", "ple_gate", "ple_norm")


def kernel(**inputs):
    n = 8
    x = np.ascontiguousarray(inputs["x"], dtype=np.float32)
    p = np.ascontiguousarray(inputs["p"], dtype=np.float32)
    per = x.shape[0] // n
    nc = build(nseq=per)
    shared = {k: np.ascontiguousarray(inputs[k], dtype=np.float32) for k in _W}
    shared["final_norm"] = np.ascontiguousarray(inputs["final_norm"], dtype=np.float32).reshape(1, D)
    in_maps = []
    for c in range(n):
        m = dict(shared)
        m["x"] = np.ascontiguousarray(x[c * per:(c + 1) * per])
        m["p"] = np.ascontiguousarray(p[:, c * per:(c + 1) * per])
        in_maps.append(m)
    res = run_bass_kernel_spmd(nc, in_maps, core_ids=list(range(n)))
    return np.concatenate([r["out"] for r in res.results], axis=0)
```

```python
import numpy as np
from contextlib import ExitStack
import concourse.bass as bass
import concourse.mybir as mybir
from concourse.bass_utils import run_bass_kernel_spmd

F32 = mybir.dt.float32
BF16 = mybir.dt.bfloat16
AF = mybir.ActivationFunctionType
ALU = mybir.AluOpType
AX = mybir.AxisListType

D = 1024
SEQ = 2048
NB = SEQ // 128
EPS = 1e-6


class Tok:
    __slots__ = ("sem", "val", "eng", "dma", "seq", "idx", "used")


class Buf:
    def __init__(self, name):
        self.name = name
        self.w = None
        self.r = {}


class TK:
    EPOCH = 12000

    def __init__(self, nc, st):
        self.nc, self.st = nc, st
        self.ops = {e: [] for e in ("pe", "act", "dve", "pool", "sp")}
        self.waited, self.waited_idx = {}, {}
        self.nsem = 0
        self.seq = 0
        self.nins = {e: 0 for e in self.ops}

    def new_sem(self, name):
        self.nsem += 1
        return self.st.enter_context(self.nc.semaphore(f"{name}_{self.nsem}"))

    def new_dma(self, name):
        return (self.new_sem(name), {"val": 0})

    def op(self, eng, fn, reads=(), writes=(), sig=True, dma=None):
        deps = []
        for b in reads:
            if b.w is not None:
                deps.append(b.w)
        for b in writes:
            if b.w is not None:
                deps.append(b.w)
            for t in b.r.values():
                if (not t.dma) and t.eng == eng and eng == "pe":
                    continue
                deps.append(t)
        ops = self.ops[eng]
        n = self.nins[eng]
        for d in deps:
            if d.dma:
                key = (eng, id(d.sem))
                if self.waited.get(key, -1) >= d.val:
                    continue
                self.waited[key] = d.val
                ops.append(("wait", d))
            elif d.eng == eng:
                if eng == "pe" or dma is not None:
                    continue
                key = (eng, d.eng)
                if self.waited_idx.get(key, -1) >= d.idx:
                    continue
                self.waited_idx[key] = d.idx
                d.used = True
                ops.append(("wait", d))
            else:
                key = (eng, d.eng)
                if self.waited_idx.get(key, -1) >= d.idx:
                    continue
                self.waited_idx[key] = d.idx
                d.used = True
                ops.append(("wait", d))
        tok = Tok()
        tok.eng = eng
        tok.dma = dma is not None
        tok.used = False
        tok.sem, tok.val = None, None
        self.seq += 1
        tok.seq = self.seq
        if dma is not None:
            sem, stt = dma
            stt["val"] += 16
            tok.sem, tok.val = sem, stt["val"]
            tok.idx = -1
            ops.append(("dma", fn, tok))
        else:
            tok.idx = n
            self.nins[eng] = n + 1
            ops.append(("op", fn, tok))
        for b in reads:
            b.r[id(tok.sem) if tok.dma else eng] = tok
        for b in writes:
            b.w = tok
            b.r = {}
        return tok

    def raw_wait(self, eng, sem, val):
        self.ops[eng].append(("rawwait", sem, val))

    def finalize(self):
        self.nsig = {}
        for eng, ops in self.ops.items():
            sem, cnt, ns = None, 0, 0
            for it in ops:
                if it[0] == "op" and it[2].used:
                    if sem is None or cnt >= self.EPOCH:
                        sem, cnt = self.new_sem("s_" + eng), 0
                    cnt += 1
                    ns += 1
                    it[2].sem, it[2].val = sem, cnt
            self.nsig[eng] = ns

    def emit(self, eng, e):
        for it in self.ops[eng]:
            if it[0] == "wait":
                e.wait_ge(it[1].sem, it[1].val)
            elif it[0] == "rawwait":
                e.wait_ge(it[1], it[2])
            elif it[0] == "dma":
                it[1](e).then_inc(it[2].sem, 16)
            else:
                ins = it[1](e)
                if it[2].used:
                    ins.then_inc(it[2].sem, 1)

    def alias(self, old, new):
        merged = {}
        for ob in old:
            for t in ([ob.w] if ob.w is not None else []) + list(ob.r.values()):
                k = id(t.sem) if t.dma else t.eng
                if k not in merged or merged[k].seq < t.seq:
                    merged[k] = t
        for nb in new:
            nb.w = None
            nb.r = dict(merged)


class T:
    def __init__(self, ap, name):
        self.ap = ap
        self.b = Buf(name)


def build(nseq=2, layers=(0, 1, 2, 3), final=True, ple=True, gla_nb=NB):
    nc = bass.Bass("TRN2", target_bir_lowering=False)
    dt_in = lambda name, shape: nc.dram_tensor(name, list(shape), F32, kind="ExternalInput").ap()
    x = dt_in("x", [nseq, SEQ, D])
    p = dt_in("p", [4, nseq, SEQ, 256])
    norm_mixer = dt_in("norm_mixer", [4, D])
    gla_w_in = dt_in("gla_w_in", [2, D, 3088])
    gla_w_a2 = dt_in("gla_w_a2", [2, 16, 512])
    gla_b_a = dt_in("gla_b_a", [2, 512])
    gla_o_norm = dt_in("gla_o_norm", [2, 256])
    gla_w_out = dt_in("gla_w_out", [2, D, D])
    fox_w_in = dt_in("fox_w_in", [2, D, 4112])
    fox_b_f = dt_in("fox_b_f", [2, 16])
    fox_q_norm = dt_in("fox_q_norm", [2, 64])
    fox_k_norm = dt_in("fox_k_norm", [2, 64])
    fox_w_out = dt_in("fox_w_out", [2, D, D])
    ple_proj = dt_in("ple_proj", [4, 256, D])
    ple_gate = dt_in("ple_gate", [4, D, D])
    ple_norm = dt_in("ple_norm", [4, D])
    final_norm = dt_in("final_norm", [1, D])
    out = nc.dram_tensor("out", [nseq, SEQ, D], F32, kind="ExternalOutput").ap()

    with ExitStack() as st:
        tk = TK(nc, st)

        def sb(name, shape, dt):
            return st.enter_context(nc.sbuf_tensor(name, list(shape), dt))

        def sbt(name, shape, dt):
            return T(sb(name, shape, dt), name)

        h = sb("h", [128, NB, D], F32)
        hB = [Buf(f"h{b}") for b in range(NB)]
        arena = sb("arena", [128, 33792], BF16)
        arena2 = sb("arena2", [128, 16384], BF16)
        gla_Win = T(arena[:, 0:8 * 3088].rearrange("p (k c) -> p k c", k=8), "gla_Win")
        Wout = T(arena[:, 25600:25600 + 8192].rearrange("p (k c) -> p k c", k=8), "Wout")
        fox_Wg = T(arena[:, 0:8192].rearrange("p (k j c) -> p k j c", k=8, j=4), "fox_Wg")
        fox_KT = T(arena[:, 8192:16384].rearrange("p (h t) -> p h t", h=4), "fox_KT")
        fox_V = T(arena[:, 16384:16384 + 16 * 4 * 65].rearrange("p (b h c) -> p b h c", b=16, h=4), "fox_V")
        fox_Wf = T(arena[:, 20736:20736 + 128].rearrange("p (k c) -> p k c", k=8), "fox_Wf")
        ple_Wg = T(arena[:, 0:8192].rearrange("p (k c) -> p k c", k=8), "ple_Wg")
        ple_Wp = T(arena[:, 8192:10240].rearrange("p (k c) -> p k c", k=2), "ple_Wp")
        KTb = [Buf(f"KT{b}") for b in range(NB)]
        Vb = [Buf(f"V{b}") for b in range(NB)]
        arena_gla = [gla_Win.b]
        arena_fox = [fox_Wg.b, fox_Wf.b] + KTb + Vb
        pT_all = arena[:, 10240:14336].rearrange("p (k t) -> p k t", k=2)
        pTB = [Buf(f"pTa{b}") for b in range(NB)]
        arena_ple = [ple_Wg.b, ple_Wp.b] + pTB
        hT = arena2[:, :].rearrange("p (k t) -> p k t", k=8)
        hTB = [Buf(f"hT{b}") for b in range(NB)]
        a2 = [0]

        def carve(name, n_bf16, dt=BF16):
            ap_ = arena2[:, a2[0]:a2[0] + n_bf16]
            a2[0] += n_bf16
            if dt == F32:
                ap_ = ap_.bitcast(F32)
            return T(ap_, name)

        xe = carve("xe", 1024, F32)
        Lbf = carve("Lbf", 512)
        E_tm = carve("E_tm", 1024, F32)
        Einv_tm = carve("Einv_tm", 1024, F32)
        q_in = carve("q_in", 512)
        k_in = carve("k_in", 512)
        q_inT = carve("q_inT", 512)
        k_inT = carve("k_inT", 512)
        v_sb = carve("v_sb", 1024)
        attnT = carve("attnT", 512)
        S_f32 = carve("S_f32", 2048, F32)
        S_bf = carve("S_bf", 1024)
        k_in_b = carve("k_in_b", 512)
        q_inT_b = carve("q_inT_b", 512)
        k_inT_b = carve("k_inT_b", 512)
        v_sb_b = carve("v_sb_b", 1024)
        k_in2, q_inT2, k_inT2, v_sb2 = [k_in, k_in_b], [q_inT, q_inT_b], [k_inT, k_inT_b], [v_sb, v_sb_b]
        gla_work = [xe, Lbf, E_tm, Einv_tm, q_in, k_in, q_inT, k_inT, v_sb, attnT, S_f32, S_bf, k_in_b, q_inT_b, k_inT_b, v_sb_b]
        assert a2[0] <= 16384

        gbc_m = sbt("gbc_m", [128, D], F32)
        gbc_p = sbt("gbc_p", [128, D], F32)
        hb = [sbt(f"hb{i}", [128, D], BF16) for i in range(2)]
        sqj = sbt("sqj", [128, D], BF16)
        hTb = [sbt(f"hTb{i}", [128, 8, 128], BF16) for i in range(2)]
        e_g = sbt("e_g", [128, D], F32)
        sg = sbt("sg", [128, D], F32)
        sgF = []
        for i_ in range(2):
            t_ = T(sg.ap[:, i_ * 256:(i_ + 1) * 256], f"sgF{i_}")
            t_.b = sg.b
            sgF.append(t_)
        og = sbt("og", [128, D], BF16)
        ogT = sbt("ogT", [128, 8, 128], BF16)
        p_b = [sbt(f"p_b{i}", [128, 256], BF16) for i in range(2)]
        pT2 = [sbt(f"pT{i}", [128, 2, 128], BF16) for i in range(2)]
        stats = sbt("stats", [128, 64], F32)
        rstd_all = sbt("rstd_all", [128, NB], F32)
        ident = sbt("ident", [128, 128], BF16)
        maskT = sbt("maskT", [128, 128], BF16)
        tri32 = sbt("tri32", [128, 128], F32)
        ones32 = sbt("ones32", [128, 128], F32)
        ones_bf = sbt("ones_bf", [128, 1], BF16)
        a_sb = sbt("a_sb", [128, 16], BF16)
        aT = sbt("aT", [17, 128], BF16)
        wa2 = sbt("wa2", [17, 512], BF16)
        gcol = sbt("gcol", [128, 2], F32)
        decay2 = [sbt(f"decay{i}", [128, 4], F32) for i in range(2)]
        decay = decay2[0]
        sso = sbt("sso", [128, 8], F32)
        kq_aug = [sbt(f"kq_aug{i}", [128, 4, 68], BF16) for i in range(2)]
        qT2 = [sbt(f"qT{i}", [68, 4, 128], BF16) for i in range(2)]
        af = [20864]

        def carve_fox(name, n_bf16, dt=BF16):
            ap_ = arena[:, af[0]:af[0] + n_bf16]
            af[0] += n_bf16
            if dt == F32:
                ap_ = ap_.bitcast(F32)
            return T(ap_, name)

        PT = [T(carve_fox(f"PT{i}", 512).ap.rearrange("p (j t) -> p j t", j=4), f"PT{i}") for i in range(3)]
        sqk2 = [carve_fox(f"sqk{i}", 512, F32) for i in range(2)]
        fst2 = [sbt(f"fst{i}", [128, 64], F32) for i in range(2)]
        kgain = sbt("kgain", [128, 64], F32)
        qgain = sbt("qgain", [128, 64], F32)
        bf_bc = sbt("bf_bc", [128, 16], F32)
        Lf = sbt("Lf", [128, 16], F32)
        Rf = sbt("Rf", [128, 16], F32)
        C_all = T(carve_fox("C_all", 512, F32).ap.rearrange("p (b c) -> p b c", b=NB), "C_all")
        Chl = T(carve_fox("Chl", 512).ap.rearrange("p (b a c) -> p b a c", b=NB, a=2), "Chl")
        Ctmp = sbt("Ctmp", [128, 16], F32)
        CB = [Buf(f"C{b}") for b in range(NB)]
        rden = sbt("rden", [128, 4], F32)
        otmp = carve_fox("otmp", 512, F32)
        assert af[0] <= 25600
        arena_fox += [t.b for t in PT + sqk2 + [otmp]] + CB

        psF = [T(st.enter_context(nc.psum_tensor(f"psF{i}", [128, 512], F32)), f"psF{i}") for i in range(6)]
        psB = [T(st.enter_context(nc.psum_tensor(f"psB{i}", [128, 1024], BF16)), f"psB{i}") for i in range(2)]
        rr = {"F": 0, "B": 0, "hb": 0, "hTb": 0, "pb": 0, "PT": 0, "aug": 0}

        def nextF(n=5):
            rr["F"] = (rr["F"] + 1) % n
            return psF[rr["F"]]

        def nextB():
            rr["B"] ^= 1
            return psB[rr["B"]]

        def rot(key, lst):
            rr[key] = (rr[key] + 1) % len(lst)
            return lst[rr[key]]

        def bl(xs):
            return [t.b if isinstance(t, T) else t for t in xs]

        def mm(o, lhsT, rhs, start, stop, reads, writes, sig=None):
            tk.op("pe", lambda e: e.matmul(o, lhsT=lhsT, rhs=rhs, start=start, stop=stop),
                  bl(reads), bl(writes), sig=(stop if sig is None else sig))

        def tr(o, i, reads, writes, sig=True):
            tk.op("pe", lambda e: e.transpose(out=o, in_=i, identity=ident.ap[:]), bl(reads) + [ident.b], bl(writes), sig=sig)

        def act(o, i, func, reads, writes, bias=None, scale=None, accum=None):
            kw = {}
            if bias is not None:
                kw["bias"] = bias
            if scale is not None:
                kw["scale"] = scale
            if accum is not None:
                kw["accum_out"] = accum
            tk.op("act", lambda e: e.activation(out=o, in_=i, func=func, **kw), bl(reads), bl(writes))

        def dve(fn, reads, writes):
            tk.op("dve", fn, bl(reads), bl(writes))

        def tt(o, a, b_, op, reads, writes):
            dve(lambda e: e.tensor_tensor(out=o, in0=a, in1=b_, op=op), reads, writes)

        def ts(o, a, s1, s2, op0, op1, reads, writes):
            if s2 is None:
                dve(lambda e: e.tensor_scalar(out=o, in0=a, scalar1=s1, scalar2=None, op0=op0), reads, writes)
            else:
                dve(lambda e: e.tensor_scalar(out=o, in0=a, scalar1=s1, scalar2=s2, op0=op0, op1=op1), reads, writes)

        def stt(o, a, s, b_, op0, op1, reads, writes):
            dve(lambda e: e.scalar_tensor_tensor(out=o, in0=a, scalar=s, in1=b_, op0=op0, op1=op1), reads, writes)

        def cp(o, i, reads, writes):
            dve(lambda e: e.tensor_copy(out=o, in_=i), reads, writes)

        def wload(o, i, writes, dmah):
            tk.op("pool", lambda e: e.dma_start(out=o, in_=i), [], bl(writes), dma=dmah)

        def wload_nc(o, i, writes, dmah):
            tk.op("pool", lambda e: e.dma_start(out=o, in_=i, allow_slow_non_contiguous=True), [], bl(writes), dma=dmah)

        dm = {}

        def dmah(name):
            if name not in dm:
                dm[name] = tk.new_dma(name)
            return dm[name]

        pl = lambda fn, reads, writes: tk.op("dve", fn, bl(reads), bl(writes))

        def pool(fn, reads, writes):
            tk.op("pool", fn, bl(reads), bl(writes))

        pool(lambda e: e.memset(ones32.ap[:], 1.0), [], [ones32])
        pool(lambda e: e.memset(tri32.ap[:], 1.0), [], [tri32])
        pool(lambda e: e.affine_select(out=tri32.ap[:], in_=tri32.ap[:], pattern=[[1, 128]], compare_op=ALU.is_ge,
                                       fill=0.0, base=0, channel_multiplier=-1), [tri32], [tri32])
        cfv = e_g.ap[:, 0:128]
        pool(lambda e: e.memset(cfv, 1.0), [], [e_g])
        pool(lambda e: e.affine_select(out=cfv, in_=cfv, pattern=[[-1, 128]], compare_op=ALU.is_equal,
                                       fill=0.0, base=0, channel_multiplier=1), [e_g], [e_g])
        cp(ident.ap[:], cfv, [e_g], [ident])
        cp(maskT.ap[:], tri32.ap[:], [tri32], [maskT])
        cp(ones_bf.ap[:], ones32.ap[:, 0:1], [ones32], [ones_bf])
        dve(lambda e: e.memset(aT.ap[:], 1.0), [], [aT])

        def pass0a(b, gbc, rstd_ap):
            hbt = hb[b % 2]
            act(sqj.ap[:], h[:, b, :], AF.Square, [hB[b]], [sqj, stats], accum=stats.ap[:, 0:1])
            act(stats.ap[:, 1:2], stats.ap[:, 0:1], AF.Ln, [stats], [stats], bias=EPS, scale=1.0 / D)
            act(rstd_ap, stats.ap[:, 1:2], AF.Exp, [stats], [rstd_all], scale=-0.5)
            tt(hbt.ap[:], h[:, b, :], gbc.ap[:], ALU.mult, [hB[b], gbc], [hbt])

        def pass0b(b, hT_dst, hT_buf):
            hbt = hb[b % 2]
            pb_ = nextB()
            for k in range(8):
                tr(pb_.ap[:, k * 128:(k + 1) * 128], hbt.ap[:, k * 128:(k + 1) * 128], [hbt], [pb_], sig=(k == 7))
            cp(hT_dst, pb_.ap[:, :].rearrange("p (k t) -> p k t", k=8), [pb_], [hT_buf])

        def pass0(b, gbc, rstd_ap, hT_dst, hT_buf):
            pass0a(b, gbc, rstd_ap)
            pass0b(b, hT_dst, hT_buf)

        def load_gbc(dst, src_row):
            wload(dst.ap[:], src_row.to_broadcast([128, D]), [dst], dmah(dst.b.name))

        def proj(hT_ap, hT_buf, W_ap_fn, ncols, Wbuf, bank):
            for k in range(8):
                mm(bank.ap[:, 0:ncols], hT_ap[:, k, :], W_ap_fn(k), k == 0, k == 7, [hT_buf, Wbuf], [bank])

        def sigmoid_act(dst, src, nrs_ap, reads, wbuf):
            act(dst, src, AF.Exp, reads, [wbuf], scale=nrs_ap)
            act(dst, dst, AF.Ln, [wbuf], [wbuf], bias=1.0)
            act(dst, dst, AF.Exp, [wbuf], [wbuf], scale=-1.0)

        def ple_pass(i, s):
            tk.alias(arena_gla + arena_fox + arena_ple, arena_ple)
            tk.alias([t.b for t in gla_work] + hTB, hTB)
            load_gbc(gbc_p, ple_norm[i:i + 1, :])
            for b in range(2):
                wload(p_b[b].ap[:], p[i, s, b * 128:(b + 1) * 128, :], [p_b[b]], dmah(p_b[b].b.name))
            wg_view = ple_gate[i].rearrange("(k p) c -> p k c", p=128)
            for k in range(8):
                wload(ple_Wg.ap[:, k, :], wg_view[:, k, :], [ple_Wg], dmah("ple_Wg"))
            wload(ple_Wp.ap[:, :, :], ple_proj[i].rearrange("(k p) c -> p k c", p=128), [ple_Wp], dmah("ple_Wp"))
            pass0a(0, gbc_p, rstd_all.ap[:, 0:1])
            for b in range(NB):
                pbt = p_b[b % 2]
                if b >= 2:
                    wload(pbt.ap[:], p[i, s, b * 128:(b + 1) * 128, :], [pbt], dmah(pbt.b.name))
                if b + 1 < NB:
                    pass0a(b + 1, gbc_p, rstd_all.ap[:, b + 1:b + 2])
                pass0b(b, hT[:, :, b * 128:(b + 1) * 128], hTB[b])
                pb_ = nextB()
                for k in range(2):
                    tr(pb_.ap[:, k * 128:(k + 1) * 128], pbt.ap[:, k * 128:(k + 1) * 128], [pbt], [pb_], sig=(k == 1))
                cp(pT_all[:, :, b * 128:(b + 1) * 128], pb_.ap[:, 0:256].rearrange("p (k t) -> p k t", k=2), [pb_], [pTB[b]])
            for b in range(NB):
                rs = rstd_all.ap[:, b:b + 1]
                hTs = hT[:, :, b * 128:(b + 1) * 128]
                for cg in range(2):
                    cs = slice(cg * 512, (cg + 1) * 512)
                    bg = nextF()
                    proj(hTs, hTB[b], lambda k: ple_Wg.ap[:, k, cs], 512, ple_Wg.b, bg)
                    act(sg.ap[:, cs], bg.ap[:], AF.Sigmoid, [bg, rstd_all], [sg], scale=rs)
                    bp = nextF()
                    for k in range(2):
                        mm(bp.ap[:], pT_all[:, k, b * 128:(b + 1) * 128], ple_Wp.ap[:, k, cs], k == 0, k == 1, [pTB[b], ple_Wp], [bp])
                    tt(e_g.ap[:, cs], bp.ap[:], sg.ap[:, cs], ALU.mult, [bp, sg], [e_g])
                    tt(h[:, b, cs], h[:, b, cs], e_g.ap[:, cs], ALU.add, [hB[b], e_g], [hB[b]])

        def gla_layer(i, s):
            j = i // 2
            tk.alias(arena_ple + arena_fox + arena_gla, arena_gla)
            tk.alias(hTB + [t.b for t in gla_work], [t.b for t in gla_work])
            load_gbc(gbc_m, norm_mixer[i:i + 1, :])
            wload(wa2.ap[0:16, :], gla_w_a2[j], [wa2], dmah("wa2"))
            wload(wa2.ap[16:17, :], gla_b_a[j:j + 1, :], [wa2], dmah("wa2"))
            tk.op("sp", lambda e: e.dma_start(out=gcol.ap[:], in_=gla_o_norm[j].rearrange("(a p) -> p a", p=128),
                                              allow_slow_non_contiguous=True), [], [gcol.b], dma=dmah("gcol"))
            wv = gla_w_in[j].rearrange("(k p) c -> p k c", p=128)
            for (c0, c1) in ((3072, 3088), (1024, 1536), (1536, 2048), (0, 512), (512, 1024), (2048, 2560), (2560, 3072)):
                for k0 in (0, 4):
                    wload(gla_Win.ap[:, k0:k0 + 4, c0:c1], wv[:, k0:k0 + 4, c0:c1], [gla_Win], dmah("gla_Win"))
            wo = gla_w_out[j].rearrange("(k p) c -> p k c", p=128)
            for k0 in (0, 4):
                wload(Wout.ap[:, k0:k0 + 4, :], wo[:, k0:k0 + 4, :], [Wout], dmah("Wout"))
            for k in range(8):
                ts(Wout.ap[:, k, :], Wout.ap[:, k, :], gcol.ap[:, (k % 2):(k % 2) + 1], None, ALU.mult, None, [Wout, gcol], [Wout])
            Win = gla_Win
            sg2 = [sg, e_g]

            def stA(b):
                par = b % 2
                rs = rstd_all.ap[:, b:b + 1]
                hTt = hTb[par]
                k_in_, q_inT_, k_inT_, v_, decay, sgt = k_in2[par], q_inT2[par], k_inT2[par], v_sb2[par], decay2[par], sg2[par]
                pass0b(b, hTt.ap[:], hTt.b)
                ba = nextF()
                proj(hTt.ap, hTt.b, lambda k: Win.ap[:, k, 3072:3088], 16, Win.b, ba)
                act(a_sb.ap[:], ba.ap[:, 0:16], AF.Copy, [ba, rstd_all], [a_sb], scale=rs)
                pb_ = nextB()
                tr(pb_.ap[0:16, 0:128], a_sb.ap[:], [a_sb], [pb_])
                cp(aT.ap[0:16, :], pb_.ap[0:16, 0:128], [pb_], [aT])
                for cg in range(2):
                    bv = nextF()
                    proj(hTt.ap, hTt.b, lambda k: Win.ap[:, k, 1024 + cg * 512:1536 + cg * 512], 512, Win.b, bv)
                    act(v_.ap[:, cg * 512:(cg + 1) * 512], bv.ap[:], AF.Copy, [bv, rstd_all], [v_], scale=rs)
                bx = nextF()
                mm(bx.ap[:], aT.ap[:], wa2.ap[:], True, True, [aT, wa2], [bx])
                bq = nextF()
                proj(hTt.ap, hTt.b, lambda k: Win.ap[:, k, 0:512], 512, Win.b, bq)
                bk_ = nextF()
                proj(hTt.ap, hTt.b, lambda k: Win.ap[:, k, 512:1024], 512, Win.b, bk_)
                act(xe.ap[:], bx.ap[:], AF.Exp, [bx], [xe], scale=-1.0)
                act(xe.ap[:], xe.ap[:], AF.Ln, [xe], [xe], bias=1.0)
                cp(Lbf.ap[:], xe.ap[:], [xe], [Lbf])
                bc = nextF()
                mm(bc.ap[:], maskT.ap[:], Lbf.ap[:], True, True, [maskT, Lbf], [bc])
                act(E_tm.ap[:], bc.ap[:], AF.Exp, [bc], [E_tm], scale=-1.0 / 16, bias=float(np.log(128.0 ** -0.5)))
                act(Einv_tm.ap[:], bc.ap[:], AF.Exp, [bc], [Einv_tm], scale=1.0 / 16)
                bd = nextF()
                for hh in range(4):
                    mm(bd.ap[:, hh:hh + 1], Lbf.ap[:, hh * 128:(hh + 1) * 128], ones_bf.ap[:], True, True, [Lbf, ones_bf], [bd], sig=(hh == 3))
                act(decay.ap[:], bd.ap[:, 0:4], AF.Exp, [bd], [decay], scale=-1.0 / 16)
                stt(q_in.ap[:], bq.ap[:], rs, E_tm.ap[:], ALU.mult, ALU.mult, [bq, rstd_all, E_tm], [q_in])
                stt(k_in_.ap[:], bk_.ap[:], rs, Einv_tm.ap[:], ALU.mult, ALU.mult, [bk_, rstd_all, Einv_tm], [k_in_])
                for (src, dst) in ((q_in, q_inT_), (k_in_, k_inT_)):
                    pb_ = nextB()
                    for hh in range(4):
                        tr(pb_.ap[:, hh * 128:(hh + 1) * 128], src.ap[:, hh * 128:(hh + 1) * 128], [src], [pb_], sig=(hh == 3))
                    cp(dst.ap[:], pb_.ap[:, 0:512], [pb_], [dst])
                for cg in range(2):
                    cs = slice(cg * 512, (cg + 1) * 512)
                    bg = nextF()
                    proj(hTt.ap, hTt.b, lambda k: Win.ap[:, k, 2048 + cg * 512:2560 + cg * 512], 512, Win.b, bg)
                    act(sgt.ap[:, cs], bg.ap[:], AF.Silu, [bg, rstd_all], [sgt], scale=rs)

            def stB(b):
                par = b % 2
                k_in_, q_inT_, k_inT_, v_, decay, sgt = k_in2[par], q_inT2[par], k_inT2[par], v_sb2[par], decay2[par], sg2[par]
                bt = nextF()
                for hh in range(4):
                    mm(bt.ap[:, hh * 128:(hh + 1) * 128], k_inT_.ap[:, hh * 128:(hh + 1) * 128], q_inT_.ap[:, hh * 128:(hh + 1) * 128],
                       True, True, [k_inT_, q_inT_], [bt], sig=(hh == 3))
                tt(attnT.ap[:].rearrange("p (h c) -> p h c", h=4), bt.ap[:].rearrange("p (h c) -> p h c", h=4),
                   maskT.ap[:].unsqueeze(1).to_broadcast([128, 4, 128]), ALU.mult, [bt, maskT], [attnT])
                bo = [nextF(), nextF()]
                for hh in range(4):
                    o_ap = bo[hh // 2].ap[:, (hh % 2) * 256:(hh % 2) * 256 + 256]
                    mm(o_ap, attnT.ap[:, hh * 128:(hh + 1) * 128], v_.ap[:, hh * 256:(hh + 1) * 256], True, b == 0,
                       [attnT, v_], [bo[hh // 2]], sig=(b == 0 and hh % 2 == 1))
                    if b > 0:
                        mm(o_ap, q_inT_.ap[:, hh * 128:(hh + 1) * 128], S_bf.ap[:, hh * 256:(hh + 1) * 256], False, True,
                           [q_inT_, S_bf], [bo[hh // 2]], sig=(hh % 2 == 1))
                bss = []
                if b < NB - 1:
                    for half in range(2):
                        bs = nextF()
                        bss.append(bs)
                        for hq in range(2):
                            hh = half * 2 + hq
                            mm(bs.ap[:, hq * 256:(hq + 1) * 256], k_in_.ap[:, hh * 128:(hh + 1) * 128], v_.ap[:, hh * 256:(hh + 1) * 256],
                               True, True, [k_in_, v_], [bs], sig=(hq == 1))
                for hh in range(4):
                    o_ap = bo[hh // 2].ap[:, (hh % 2) * 256:(hh % 2) * 256 + 256]
                    act(sqj.ap[:, 0:256], o_ap, AF.Square, [bo[hh // 2]], [sqj, sso], accum=sso.ap[:, hh:hh + 1])
                act(sso.ap[:, 4:8], sso.ap[:, 0:4], AF.Ln, [sso], [sso], bias=EPS, scale=1.0 / 256)
                act(sso.ap[:, 0:4], sso.ap[:, 4:8], AF.Exp, [sso], [sso], scale=-0.5)
                for hh in range(4):
                    o_ap = bo[hh // 2].ap[:, (hh % 2) * 256:(hh % 2) * 256 + 256]
                    stt(og.ap[:, hh * 256:(hh + 1) * 256], o_ap, sso.ap[:, hh:hh + 1], sgt.ap[:, hh * 256:(hh + 1) * 256],
                        ALU.mult, ALU.mult, [bo[hh // 2], sso, sgt], [og])
                out_proj(b, range(8))
                if b + 2 < gla_nb:
                    pass0a(b + 2, gbc_m, rstd_all.ap[:, b + 2:b + 3])
                if b < NB - 1:
                    for half in range(2):
                        bs = bss[half]
                        Sv = S_f32.ap[:, half * 512:(half + 1) * 512]
                        if b == 0:
                            cp(Sv, bs.ap[:], [bs], [S_f32])
                        else:
                            tt(Sv, Sv, bs.ap[:], ALU.add, [bs, S_f32], [S_f32])
                        for hq in range(2):
                            hh = half * 2 + hq
                            Sh = S_f32.ap[:, hh * 256:(hh + 1) * 256]
                            ts(Sh, Sh, decay.ap[:, hh:hh + 1], None, ALU.mult, None, [S_f32, decay], [S_f32])
                    act(S_bf.ap[:], S_f32.ap[:], AF.Copy, [S_f32], [S_bf])

            pass0a(0, gbc_m, rstd_all.ap[:, 0:1])
            stA(0)
            if gla_nb > 1:
                pass0a(1, gbc_m, rstd_all.ap[:, 1:2])
            for b in range(gla_nb):
                if b + 1 < gla_nb:
                    stA(b + 1)
                stB(b)

        def out_proj(b, chunks, nf=5):
            chunks = list(chunks)
            n = len(chunks)
            pb_ = nextB()
            for ci, k in enumerate(chunks):
                tr(pb_.ap[:, ci * 128:(ci + 1) * 128], og.ap[:, ci * 128:(ci + 1) * 128], [og], [pb_], sig=(ci == n - 1))
            dve(lambda e: e.tensor_copy(out=ogT.ap[:, 0:n, :], in_=pb_.ap[:, 0:n * 128].rearrange("p (k t) -> p k t", k=n)), [pb_], [ogT])
            for cg in range(2):
                cs = slice(cg * 512, (cg + 1) * 512)
                by = nextF(nf)
                for ci, k in enumerate(chunks):
                    mm(by.ap[:], ogT.ap[:, ci, :], Wout.ap[:, k, cs], ci == 0, ci == n - 1, [ogT, Wout], [by])
                tt(h[:, b, cs], h[:, b, cs], by.ap[:], ALU.add, [hB[b], by], [hB[b]])

        def fox_layer(i, s):
            j = i // 2
            tk.alias(arena_ple + arena_gla + arena_fox, arena_fox)
            tk.alias([t.b for t in gla_work] + hTB, hTB)
            wv = fox_w_in[j].rearrange("(k p) c -> p k c", p=128)
            wo = fox_w_out[j].rearrange("(k p) c -> p k c", p=128)
            load_gbc(gbc_m, norm_mixer[i:i + 1, :])
            wload(fox_Wf.ap[:, :, :], wv[:, :, 4096:4112], [fox_Wf], dmah("fox_Wf"))
            wload(bf_bc.ap[:], fox_b_f[j:j + 1, :].to_broadcast([128, 16]), [bf_bc], dmah("bf_bc"))
            wload(kgain.ap[:], fox_k_norm[j:j + 1, :].to_broadcast([128, 64]), [kgain], dmah("kgain"))
            wload(qgain.ap[:], fox_q_norm[j:j + 1, :].to_broadcast([128, 64]), [qgain], dmah("qgain"))
            for k0 in (0, 4):
                wload(Wout.ap[:, k0:k0 + 4, :], wo[:, k0:k0 + 4, :], [Wout], dmah("Wout"))
            ts(qgain.ap[:], qgain.ap[:], 0.125, None, ALU.mult, None, [qgain], [qgain])
            COLB = (0, 3072, 1024, 2048)

            def load_group(G):
                for jj in range(4):
                    c0 = COLB[jj] + G * 256
                    wload(fox_Wg.ap[:, :, jj, :], wv[:, :, c0:c0 + 256], [fox_Wg], dmah("fox_Wg"))

            load_group(0)
            dve(lambda e: e.memset(Rf.ap[:], 0.0), [], [Rf])
            pass0a(0, gbc_m, rstd_all.ap[:, 0:1])
            for b in range(NB):
                rs = rstd_all.ap[:, b:b + 1]
                if b + 1 < NB:
                    pass0a(b + 1, gbc_m, rstd_all.ap[:, b + 1:b + 2])
                pass0b(b, hT[:, :, b * 128:(b + 1) * 128], hTB[b])
                bf_ = nextF(4)
                proj(hT[:, :, b * 128:(b + 1) * 128], hTB[b], lambda k: fox_Wf.ap[:, k, :], 16, fox_Wf.b, bf_)
                stt(Lf.ap[:], bf_.ap[:, 0:16], rs, bf_bc.ap[:], ALU.mult, ALU.add, [bf_, rstd_all, bf_bc], [Lf])
                act(Lf.ap[:], Lf.ap[:], AF.Exp, [Lf], [Lf], scale=-1.0)
                act(Lf.ap[:], Lf.ap[:], AF.Ln, [Lf], [Lf], bias=1.0)
                bc = nextF(4)
                mm(bc.ap[:, 0:16], tri32.ap[:], Lf.ap[:], True, False, [tri32, Lf], [bc], sig=False)
                mm(bc.ap[:, 0:16], ones32.ap[:], Rf.ap[:], False, True, [ones32, Rf], [bc])
                cp(C_all.ap[:, b, :], bc.ap[:, 0:16], [bc], [CB[b]])
                tt(Rf.ap[:], Rf.ap[:], Lf.ap[:], ALU.add, [Rf, Lf], [Rf])
                cp(Chl.ap[:, b, 0, :], C_all.ap[:, b, :], [CB[b]], [CB[b]])
                tt(Ctmp.ap[:], C_all.ap[:, b, :], Chl.ap[:, b, 0, :], ALU.subtract, [CB[b]], [Ctmp])
                cp(Chl.ap[:, b, 1, :], Ctmp.ap[:], [Ctmp], [CB[b]])
            dve(lambda e: e.memset(fox_V.ap[:, :, :, 64:65], 1.0), [], Vb)

            def qk_norm_aug(bank, rs, b, G, gain, is_q, aug, par):
                sqk = sqk2[par]
                fst = fst2[par]
                act(sqk.ap[:], bank.ap[:, 0:256], AF.Square, [bank], [sqk])
                yield
                dve(lambda e: e.reduce_sum(out=fst.ap[:, 0:4], in_=sqk.ap[:].rearrange("p (h d) -> p h d", h=4), axis=AX.X), [sqk], [fst])
                tt(fst.ap[:, 8:9], rs, rs, ALU.mult, [rstd_all], [fst])
                ts(fst.ap[:, 4:8], fst.ap[:, 0:4], fst.ap[:, 8:9], None, ALU.mult, None, [fst], [fst])
                yield
                act(fst.ap[:, 12:16], fst.ap[:, 4:8], AF.Ln, [fst], [fst], bias=EPS, scale=1.0 / 64)
                act(fst.ap[:, 16:20], fst.ap[:, 12:16], AF.Exp, [fst], [fst], scale=-0.5)
                yield
                ts(fst.ap[:, 20:24], fst.ap[:, 16:20], rs, None, ALU.mult, None, [fst, rstd_all], [fst])
                tt(sqk.ap[:].rearrange("p (h d) -> p h d", h=4), bank.ap[:, 0:256].rearrange("p (h d) -> p h d", h=4),
                   fst.ap[:, 20:24].unsqueeze(2).to_broadcast([128, 4, 64]), ALU.mult, [bank, fst], [sqk])
                tt(aug.ap[:, :, 0:64], sqk.ap[:].rearrange("p (h d) -> p h d", h=4),
                   gain.ap[:].unsqueeze(1).to_broadcast([128, 4, 64]), ALU.mult, [sqk, gain], [aug])
                hs = slice(G * 4, G * 4 + 4)
                if is_q:
                    ts(aug.ap[:, :, 64:66], Chl.ap[:, b, :, hs].rearrange("p a h -> p h a"), -1.0, None, ALU.mult, None, [CB[b]], [aug])
                    dve(lambda e: e.memset(aug.ap[:, :, 66:68], 1.0), [], [aug])
                else:
                    dve(lambda e: e.memset(aug.ap[:, :, 64:66], 1.0), [], [aug])
                    cp(aug.ap[:, :, 66:68], Chl.ap[:, b, :, hs].rearrange("p a h -> p h a"), [CB[b]], [aug])
                yield

            for G in range(4):
                if G > 0:
                    load_group(G)

                def p1A(b):
                    rs = rstd_all.ap[:, b:b + 1]
                    hTs = hT[:, :, b * 128:(b + 1) * 128]
                    bk_ = nextF(4)
                    proj(hTs, hTB[b], lambda k: fox_Wg.ap[:, k, 2:4, :], 512, fox_Wg.b, bk_)
                    for _ in qk_norm_aug(bk_, rs, b, G, kgain, False, kq_aug[b % 2], b % 2):
                        pass
                    act(fox_V.ap[:, b, :, 0:64], bk_.ap[:, 256:512].rearrange("p (h d) -> p h d", h=4), AF.Copy, [bk_, rstd_all], [Vb[b]], scale=rs)

                def p1B(b):
                    aug = kq_aug[b % 2]
                    pb_ = nextB()
                    for hh in range(4):
                        tr(pb_.ap[0:68, hh * 128:(hh + 1) * 128], aug.ap[:, hh, :], [aug], [pb_], sig=(hh == 3))
                    cp(fox_KT.ap[0:68, :, b * 128:(b + 1) * 128], pb_.ap[0:68, 0:512].rearrange("p (h t) -> p h t", h=4), [pb_], [KTb[b]])

                p1A(0)
                for b in range(NB):
                    if b + 1 < NB:
                        p1A(b + 1)
                    p1B(b)

                def front(b):
                    rs = rstd_all.ap[:, b:b + 1]
                    hTs = hT[:, :, b * 128:(b + 1) * 128]
                    bq = psF[3]
                    proj(hTs, hTB[b], lambda k: fox_Wg.ap[:, k, 0:2, :], 512, fox_Wg.b, bq)
                    yield
                    yield from qk_norm_aug(bq, rs, b, G, qgain, True, kq_aug[b % 2], b % 2)
                    ts(stats.ap[:, 2:3], rs, -1.0, None, ALU.mult, None, [rstd_all], [stats])
                    dst = e_g.ap[:, 0:256]
                    act(dst, bq.ap[:, 256:512], AF.Exp, [bq, stats], [e_g], scale=stats.ap[:, 2:3])
                    yield
                    act(dst, dst, AF.Ln, [e_g], [e_g], bias=1.0)
                    yield
                    act(dst, dst, AF.Exp, [e_g], [e_g], scale=-1.0)
                    yield
                    sgt = sgF[b % 2]
                    stt(sgt.ap[:], bq.ap[:, 256:512], rs, e_g.ap[:, 0:256], ALU.mult, ALU.mult, [bq, rstd_all, e_g], [sgt])

                def frontB(b):
                    aug = kq_aug[b % 2]
                    qTt = qT2[b % 2]
                    pb_ = nextB()
                    for hh in range(4):
                        tr(pb_.ap[0:68, hh * 128:(hh + 1) * 128], aug.ap[:, hh, :], [aug], [pb_], sig=(hh == 3))
                    cp(qTt.ap[:], pb_.ap[0:68, 0:512].rearrange("p (h t) -> p h t", h=4), [pb_], [qTt])

                def attn(b, filler):
                    qTt = qT2[b % 2]
                    oacc = psF[4 + (b % 2)]
                    banks = {}

                    def S(jb):
                        bs = nextF(3)
                        banks[jb] = bs
                        for hh in range(4):
                            mm(bs.ap[:, hh * 128:(hh + 1) * 128], fox_KT.ap[0:68, hh, jb * 128:(jb + 1) * 128], qTt.ap[:, hh, :],
                               True, True, [KTb[jb], qTt], [bs], sig=(hh == 3))

                    def P(jb):
                        bs = banks.pop(jb)
                        pt = rot("PT", PT)
                        act(pt.ap[:], bs.ap[:].rearrange("p (j t) -> p j t", j=4), AF.Exp, [bs], [pt])
                        if jb == b:
                            tt(pt.ap[:], pt.ap[:], maskT.ap[:].unsqueeze(1).to_broadcast([128, 4, 128]), ALU.mult, [pt, maskT], [pt])
                        for hh in range(4):
                            mm(oacc.ap[:, hh * 65:(hh + 1) * 65], pt.ap[:, hh, :], fox_V.ap[:, jb, hh, :], (jb == 0 and hh == 0), (jb == b and hh == 3),
                               [pt, Vb[jb]], [oacc], sig=(hh == 3))

                    LA = 2
                    for jb in range(min(LA, b + 1)):
                        S(jb)
                    for jb in range(b + 1):
                        if jb + LA <= b:
                            S(jb + LA)
                        P(jb)
                        if filler is not None:
                            next(filler, None)
                    if filler is not None:
                        for _ in filler:
                            pass

                def backD(b):
                    oacc = psF[4 + (b % 2)]
                    o3 = oacc.ap[:, 0:260].rearrange("p (h c) -> p h c", h=4)
                    dve(lambda e: e.reciprocal(out=rden.ap[:].unsqueeze(2), in_=o3[:, :, 64:65]), [oacc], [rden])
                    tt(otmp.ap[:].rearrange("p (h d) -> p h d", h=4), o3[:, :, 0:64],
                       rden.ap[:].unsqueeze(2).to_broadcast([128, 4, 64]), ALU.mult, [oacc, rden], [otmp])
                    tt(og.ap[:, 0:256], otmp.ap[:], sgF[b % 2].ap[:], ALU.mult, [otmp, sgF[b % 2]], [og])

                def backP(b):
                    out_proj(b, [2 * G, 2 * G + 1], nf=4)

                for _ in front(0):
                    pass
                frontB(0)
                for b in range(NB):
                    if b >= 1:
                        backD(b - 1)
                    attn(b, front(b + 1) if b + 1 < NB else None)
                    if b + 1 < NB:
                        frontB(b + 1)
                    if b >= 1:
                        backP(b - 1)
                backD(NB - 1)
                backP(NB - 1)

        def final_pass(s):
            load_gbc(gbc_m, final_norm[0:1, :])
            stg = [e_g, sg]
            for b in range(NB):
                act(sqj.ap[:], h[:, b, :], AF.Square, [hB[b]], [sqj, stats], accum=stats.ap[:, 0:1])
                act(stats.ap[:, 1:2], stats.ap[:, 0:1], AF.Ln, [stats], [stats], bias=EPS, scale=1.0 / D)
                act(stats.ap[:, 3:4], stats.ap[:, 1:2], AF.Exp, [stats], [stats], scale=-0.5)
                t_ = stg[b % 2]
                stt(t_.ap[:], h[:, b, :], stats.ap[:, 3:4], gbc_m.ap[:], ALU.mult, ALU.mult, [hB[b], stats, gbc_m], [t_])
                tk.op("sp", lambda e, t_=t_, b=b: e.dma_start(out=out[s, b * 128:(b + 1) * 128, :], in_=t_.ap[:]),
                      [t_.b], [], dma=dmah(f"st{b % 2}"))

        def raw_store(s):
            for b in range(NB):
                tk.op("sp", lambda e, b=b: e.dma_start(out=out[s, b * 128:(b + 1) * 128, :], in_=h[:, b, :]),
                      [hB[b]], [], dma=dmah(f"st{b % 2}"))

        for s in range(nseq):
            for b in range(NB):
                tk.op("sp", lambda e, b=b, s=s: e.dma_start(out=h[:, b, :], in_=x[s, b * 128:(b + 1) * 128, :]), [], [hB[b]], dma=dmah("xload"))
            last = hB[NB - 1].w
            for b in range(NB):
                hB[b].w = last
            for i in layers:
                if i % 2 == 0:
                    gla_layer(i, s)
                else:
                    fox_layer(i, s)
                if ple:
                    ple_pass(i, s)
            if final:
                final_pass(s)
            else:
                raw_store(s)
        for name in ("st0", "st1"):
            sem, stt_ = dmah(name)
            tk.raw_wait("sp", sem, stt_["val"])
        tk.finalize()

        block = st.enter_context(nc.Block())

        @block.tensor
        def _(e):
            tk.emit("pe", e)

        @block.scalar
        def _(e):
            tk.emit("act", e)

        @block.vector
        def _(e):
            tk.emit("dve", e)

        @block.gpsimd
        def _(e):
            tk.emit("pool", e)

        @block.sync
        def _(e):
            tk.emit("sp", e)
        build.stats = (dict(tk.nins), dict(tk.nsig))
    return nc


_W = ("norm_mixer", "gla_w_in", "gla_w_a2", "gla_b_a", "gla_o_norm", "gla_w_out", "fox_w_in", "fox_b_f",
      "fox_q_norm", "fox_k_norm", "fox_w_out", "ple_proj", "ple_gate", "ple_norm")


def kernel(**inputs):
    n = 8
    x = np.ascontiguousarray(inputs["x"], dtype=np.float32)
    p = np.ascontiguousarray(inputs["p"], dtype=np.float32)
    per = x.shape[0] // n
    nc = build(nseq=per)
    shared = {k: np.ascontiguousarray(inputs[k], dtype=np.float32) for k in _W}
    shared["final_norm"] = np.ascontiguousarray(inputs["final_norm"], dtype=np.float32).reshape(1, D)
    in_maps = []
    for c in range(n):
        m = dict(shared)
        m["x"] = np.ascontiguousarray(x[c * per:(c + 1) * per])
        m["p"] = np.ascontiguousarray(p[:, c * per:(c + 1) * per])
        in_maps.append(m)
    res = run_bass_kernel_spmd(nc, in_maps, core_ids=list(range(n)))
    return np.concatenate([r["out"] for r in res.results], axis=0)
```
